# Optimizing a Trainium2 kernel written in Bass

```python
import jax, jax.numpy as jnp
from jax import lax
import numpy as np

D_MODEL = 1024
BATCH = 2
SEQ = 16384
DEPTH = 4

CHUNK = 64
N_A = DEPTH // 2
N_B = DEPTH - N_A
RET_HEADS = 4
RET_QK_DIM = D_MODEL // RET_HEADS
RET_V_DIM = 2 * D_MODEL // RET_HEADS
ROPE_BASE = 10000.0
FOX_HEADS = 16
FOX_HEAD_DIM = D_MODEL // FOX_HEADS
Q_BLOCK = 128
FORGET_BIAS = 3.0
D_FF = 2816
N_EXPERTS = 8
TOP_K = 2
D_FF_EXPERT = 3584
N_DENSE = (DEPTH + 1) // 2
N_MOE = DEPTH // 2
EPS = 1e-6

kernel_name = "yoco_retention_fox_moe_trunk"


def rmsnorm(x, g):
    xf = x.astype(jnp.float32)
    y = xf * lax.rsqrt(jnp.mean(xf * xf, axis=-1, keepdims=True) + EPS)
    return (y * g.astype(jnp.float32)).astype(x.dtype)


def modulate(h, shift, scale):
    return h * (1.0 + scale[:, None, :]) + shift[:, None, :]


def rotary(x, pos):
    d = x.shape[-1]
    inv = 1.0 / (ROPE_BASE ** (jnp.arange(0, d, 2, dtype=jnp.float32) / d))
    ang = pos.astype(jnp.float32)[:, None] * inv[None, :]
    cos = jnp.cos(ang)[None, :, None, :]
    sin = jnp.sin(ang)[None, :, None, :]
    x1 = x[..., : d // 2].astype(jnp.float32)
    x2 = x[..., d // 2:].astype(jnp.float32)
    return jnp.concatenate([x1 * cos - x2 * sin, x2 * cos + x1 * sin], axis=-1).astype(x.dtype)


def retention(h, w_in, w_o):
    B, S, D = h.shape
    nc = S // CHUNK
    proj = h @ w_in
    q, k, v, g = jnp.split(proj, [D, 2 * D, 4 * D], axis=-1)
    pos = jnp.arange(S)
    q = rotary(q.reshape(B, S, RET_HEADS, RET_QK_DIM), pos)
    k = rotary(k.reshape(B, S, RET_HEADS, RET_QK_DIM), pos) * (RET_QK_DIM ** -0.5)
    v = v.reshape(B, S, RET_HEADS, RET_V_DIM)

    def to_chunks(t):
        return t.reshape(B, nc, CHUNK, RET_HEADS, -1).transpose(1, 0, 3, 2, 4).astype(jnp.float32)

    log_g = jnp.log(1.0 - 2.0 ** (-5.0 - jnp.arange(RET_HEADS, dtype=jnp.float32)))
    idx = jnp.arange(CHUNK, dtype=jnp.float32)
    intra_decay = jnp.exp(log_g[:, None, None] * jnp.abs(idx[:, None] - idx[None, :]))
    q_decay = jnp.exp(log_g[:, None] * (idx[None, :] + 1.0))[None, :, :, None]
    k_decay = jnp.exp(log_g[:, None] * (CHUNK - 1.0 - idx[None, :]))[None, :, :, None]
    chunk_decay = jnp.exp(log_g * CHUNK)[None, :, None, None]

    def step(state, qkv):
        qc, kc, vc = qkv
        scores = jnp.einsum('bhnd,bhmd->bhnm', qc, kc) * intra_decay
        o = (jnp.einsum('bhnm,bhmv->bhnv', scores, vc)
             + jnp.einsum('bhnd,bhdv->bhnv', qc, state) * q_decay)
        state = state * chunk_decay + jnp.einsum('bhmd,bhmv->bhdv', kc * k_decay, vc)
        return state, o

    state0 = jnp.zeros((B, RET_HEADS, RET_QK_DIM, RET_V_DIM), jnp.float32)
    _, o = lax.scan(step, state0, (to_chunks(q), to_chunks(k), to_chunks(v)))
    o = o.transpose(1, 0, 3, 2, 4).reshape(B, S, RET_HEADS, RET_V_DIM)
    mu = jnp.mean(o, axis=-1, keepdims=True)
    var = jnp.mean(jnp.square(o - mu), axis=-1, keepdims=True)
    y = ((o - mu) * lax.rsqrt(var + EPS)).reshape(B, S, 2 * D).astype(h.dtype)
    return (jax.nn.silu(g) * y) @ w_o


def fox_shared_kv(hn, w_kv, w_f, b_f):
    B, S, D = hn.shape
    k, v = jnp.split(hn @ w_kv, 2, axis=-1)
    k = k.reshape(B, S, FOX_HEADS, FOX_HEAD_DIM).transpose(0, 2, 1, 3)
    v = v.reshape(B, S, FOX_HEADS, FOX_HEAD_DIM).transpose(0, 2, 1, 3)
    log_f = jax.nn.log_sigmoid((hn @ w_f).astype(jnp.float32) + b_f.astype(jnp.float32))
    F = jnp.cumsum(log_f, axis=1).transpose(0, 2, 1)
    return k, v, F


def forgetting_attention(hn, w_qg, w_o, k, v, F):
    B, S, D = hn.shape
    nb = S // Q_BLOCK
    q, g = jnp.split(hn @ w_qg, 2, axis=-1)
    qb_all = q.reshape(B, nb, Q_BLOCK, FOX_HEADS, FOX_HEAD_DIM).transpose(1, 0, 3, 2, 4)
    Fq_all = F.reshape(B, FOX_HEADS, nb, Q_BLOCK).transpose(2, 0, 1, 3)
    key_pos = jnp.arange(S)
    scale = FOX_HEAD_DIM ** -0.5

    def block(args):
        qb, Fqb, i = args
        q_pos = i * Q_BLOCK + jnp.arange(Q_BLOCK)
        s = (jnp.einsum('bhqd,bhkd->bhqk', qb, k).astype(jnp.float32) * scale
             + (Fqb[..., :, None] - F[:, :, None, :]))
        s = jnp.where(key_pos[None, :] <= q_pos[:, None], s, -jnp.inf)
        p = jax.nn.softmax(s, axis=-1)
        return jnp.einsum('bhqk,bhkd->bhqd', p.astype(v.dtype), v)

    o = lax.map(block, (qb_all, Fq_all, jnp.arange(nb)))
    o = o.transpose(1, 0, 3, 2, 4).reshape(B, S, D)
    return (o * jax.nn.sigmoid(g)) @ w_o


def swiglu(h, w_gate, w_up, w_down):
    return (jax.nn.silu(h @ w_gate) * (h @ w_up)) @ w_down


def moe_swiglu(h, w_router, b_router, w_gate, w_up, w_down):
    logits = (h @ w_router).astype(jnp.float32) + b_router.astype(jnp.float32)
    top_val, top_idx = lax.top_k(logits, TOP_K)
    top_w = jax.nn.softmax(top_val, axis=-1)
    gates = jnp.sum(jax.nn.one_hot(top_idx, N_EXPERTS, dtype=jnp.float32) * top_w[..., None], axis=-2)
    gates = gates.astype(h.dtype)
    out = jnp.zeros_like(h)
    for e in range(N_EXPERTS):
        out = out + gates[..., e:e + 1] * swiglu(h, w_gate[e], w_up[e], w_down[e])
    return out


def setup_inputs(seed: int = 0) -> dict:
    key = jax.random.key(seed)
    ks = jax.random.split(key, 32)
    D = D_MODEL
    f32 = jnp.float32

    def nrm(k, shape):
        return jax.random.normal(k, shape, f32)

    def w(k, shape, fan_in, scale=1.0):
        return nrm(k, shape) * (scale * fan_in ** -0.5)

    return {
        "x": nrm(ks[0], (BATCH, SEQ, D)),
        "c": nrm(ks[1], (BATCH, D)),
        "ada_w": w(ks[2], (DEPTH, D, 6 * D), D, 0.5),
        "ada_b": 0.02 * nrm(ks[3], (DEPTH, 6 * D)),
        "norm_g": 1.0 + 0.01 * nrm(ks[4], (DEPTH, 2, D)),
        "ret_w_in": w(ks[5], (N_A, D, 6 * D), D),
        "ret_w_o": w(ks[6], (N_A, 2 * D, D), 2 * D),
        "kv_ada_w": w(ks[7], (D, 2 * D), D, 0.5),
        "kv_ada_b": 0.02 * nrm(ks[8], (2 * D,)),
        "kv_norm_g": 1.0 + 0.01 * nrm(ks[9], (D,)),
        "fox_w_kv": w(ks[10], (D, 2 * D), D),
        "fox_w_f": w(ks[11], (D, FOX_HEADS), D),
        "fox_b_f": FORGET_BIAS + 0.1 * nrm(ks[12], (FOX_HEADS,)),
        "fox_w_qg": w(ks[13], (N_B, D, 2 * D), D),
        "fox_w_o": w(ks[14], (N_B, D, D), D),
        "ffn_w_gate": w(ks[15], (N_DENSE, D, D_FF), D),
        "ffn_w_up": w(ks[16], (N_DENSE, D, D_FF), D),
        "ffn_w_down": w(ks[17], (N_DENSE, D_FF, D), D_FF),
        "router_w": w(ks[18], (N_MOE, D, N_EXPERTS), D),
        "router_b": 0.01 * nrm(ks[19], (N_MOE, N_EXPERTS)),
        "moe_w_gate": w(ks[20], (N_MOE, N_EXPERTS, D, D_FF_EXPERT), D),
        "moe_w_up": w(ks[21], (N_MOE, N_EXPERTS, D, D_FF_EXPERT), D),
        "moe_w_down": w(ks[22], (N_MOE, N_EXPERTS, D_FF_EXPERT, D), D_FF_EXPERT),
        "final_ada_w": w(ks[23], (D, 2 * D), D, 0.5),
        "final_ada_b": 0.02 * nrm(ks[24], (2 * D,)),
        "final_norm_g": 1.0 + 0.01 * nrm(ks[25], (D,)),
    }


def reference(x, c, ada_w, ada_b, norm_g, ret_w_in, ret_w_o, kv_ada_w, kv_ada_b, kv_norm_g,
              fox_w_kv, fox_w_f, fox_b_f, fox_w_qg, fox_w_o, ffn_w_gate, ffn_w_up, ffn_w_down,
              router_w, router_b, moe_w_gate, moe_w_up, moe_w_down, final_ada_w, final_ada_b,
              final_norm_g):
    c_act = jax.nn.silu(c)
    h = x
    shared = None
    for l in range(DEPTH):
        sh1, sc1, g1, sh2, sc2, g2 = jnp.split(c_act @ ada_w[l] + ada_b[l], 6, axis=-1)
        if l == N_A:
            kv_sh, kv_sc = jnp.split(c_act @ kv_ada_w + kv_ada_b, 2, axis=-1)
            hn_kv = modulate(rmsnorm(h, kv_norm_g), kv_sh, kv_sc)
            shared = fox_shared_kv(hn_kv, fox_w_kv, fox_w_f, fox_b_f)
        a = modulate(rmsnorm(h, norm_g[l, 0]), sh1, sc1)
        if l < N_A:
            mix = retention(a, ret_w_in[l], ret_w_o[l])
        else:
            j = l - N_A
            mix = forgetting_attention(a, fox_w_qg[j], fox_w_o[j], shared[0], shared[1], shared[2])
        h = h + g1[:, None, :] * mix
        m = modulate(rmsnorm(h, norm_g[l, 1]), sh2, sc2)
        if l % 2 == 0:
            i = l // 2
            ff = swiglu(m, ffn_w_gate[i], ffn_w_up[i], ffn_w_down[i])
        else:
            i = l // 2
            ff = moe_swiglu(m, router_w[i], router_b[i], moe_w_gate[i], moe_w_up[i], moe_w_down[i])
        h = h + g2[:, None, :] * ff
    f_sh, f_sc = jnp.split(c_act @ final_ada_w + final_ada_b, 2, axis=-1)
    return modulate(rmsnorm(h, final_norm_g), f_sh, f_sc)
```

```python
import contextlib
import numpy as np
import ml_dtypes
import concourse.bass as bass
import concourse.mybir as mybir
from concourse.bass_utils import run_bass_kernel_spmd

F32 = mybir.dt.float32
BF16 = mybir.dt.bfloat16
ALU = mybir.AluOpType
AF = mybir.ActivationFunctionType
AX = mybir.AxisListType
NPBF = ml_dtypes.bfloat16

ENGS = ["pe", "act", "dve", "pool", "sp"]
SEG = 6000
SELF_SYNC = True


class Res:
    _n = 0

    def __init__(self, name="r"):
        Res._n += 1
        self.name = f"{name}_{Res._n}"
        self.last_w = None
        self.readers = []


class Prog:
    def __init__(self, nc):
        self.nc = nc
        self.ops = {e: [] for e in ENGS}
        self.seq = {e: 0 for e in ENGS}
        self.known = {e: {} for e in ENGS}
        self.semkeys = {}
        self.dma_cnt = {}
        self.stack = contextlib.ExitStack()
        self.out_waits = []
        self._nt = 0

    def sbuf(self, shape, dtype, name=None):
        self._nt += 1
        return self.stack.enter_context(
            self.nc.sbuf_tensor(name or f"sb{self._nt}", list(shape), dtype))

    def psum(self, shape, dtype=F32, name=None):
        self._nt += 1
        return self.stack.enter_context(
            self.nc.psum_tensor(name or f"ps{self._nt}", list(shape), dtype))

    def _deps(self, eng, reads, writes):
        deps = []
        for r in reads:
            if r.last_w is not None:
                deps.append(r.last_w)
        for w in writes:
            if w.last_w is not None:
                deps.append(w.last_w)
            deps.extend(w.readers)
        waits = {}
        for key, val, deng in deps:
            if deng == eng and (eng in ("pe",) or not SELF_SYNC):
                continue
            if self.known[eng].get(key, 0) >= val:
                continue
            if waits.get(key, 0) < val:
                waits[key] = val
        for k, v in waits.items():
            self.known[eng][k] = v
            self.semkeys[k] = None
        return list(waits.items())

    def op(self, eng, meth, *args, reads=(), writes=(), **kw):
        waits = self._deps(eng, reads, writes)
        s = self.seq[eng]
        self.seq[eng] += 1
        key = ("eng", eng, s // SEG)
        val = s % SEG + 1
        self.semkeys[key] = None
        tok = (key, val, eng)
        for r in reads:
            r.readers.append(tok)
        for w in writes:
            w.last_w = tok
            w.readers = []
        self.ops[eng].append((waits, meth, args, kw, (key, 1)))
        return tok

    def dma(self, q, out, in_, reads=(), writes=(), is_output=False, **kw):
        waits = self._deps(q, reads, writes)
        anchor = writes[0] if writes else reads[0]
        key = ("dma", anchor.name, "w" if writes else "r")
        self.dma_cnt[key] = self.dma_cnt.get(key, 0) + 16
        val = self.dma_cnt[key]
        self.semkeys[key] = None
        tok = (key, val, "dma")
        for r in reads:
            r.readers.append(tok)
        for w in writes:
            w.last_w = tok
            w.readers = []
        kw = dict(kw)
        kw["out"] = out
        kw["in_"] = in_
        self.ops[q].append((waits, "dma_start", (), kw, (key, 16)))
        if is_output:
            self.out_waits.append((key, val))
        return tok

    def mm(self, out, lhsT, rhs, start, stop, reads, writes):
        return self.op("pe", "matmul", out, lhsT, rhs, start=start, stop=stop,
                       reads=reads, writes=writes)

    def emit(self):
        nc = self.nc
        sems = {}
        for i, k in enumerate(self.semkeys):
            sems[k] = self.stack.enter_context(nc.semaphore(f"s{i}"))
        fin = {}
        for k, v in self.out_waits:
            fin[k] = max(fin.get(k, 0), v)
        ops = self.ops

        def run(engh, ename):
            for waits, meth, args, kw, (ikey, inc) in ops[ename]:
                for k, v in waits:
                    engh.wait_ge(sems[k], v)
                getattr(engh, meth)(*args, **kw).then_inc(sems[ikey], inc)
            if ename == "sp":
                for k, v in fin.items():
                    engh.wait_ge(sems[k], v)

        with nc.Block() as block:
            @block.sync
            def _(e):
                run(e, "sp")

            @block.scalar
            def _(e):
                run(e, "act")

            @block.vector
            def _(e):
                run(e, "dve")

            @block.gpsimd
            def _(e):
                run(e, "pool")

            @block.tensor
            def _(e):
                run(e, "pe")
        self.stack.close()
        print("ops:", {e: len(v) for e, v in ops.items()}, "sems:", len(sems), flush=True)
        return nc


class PsumPool:
    def __init__(self, P, n):
        self.banks = [(P.psum([128, 512], F32), Res("psb")) for _ in range(n)]
        self.i = 0

    def get(self):
        b = self.banks[self.i % len(self.banks)]
        self.i += 1
        return b


D = 1024
EPS = 1e-6
TC = 4096
TT = 512


def build_mod():
    nc = bass.Bass("TRN2", target_bir_lowering=False)
    cT = nc.dram_tensor("cT", [128, 16], F32, kind="ExternalInput").ap()
    W = nc.dram_tensor("W", [1024, 3584], F32, kind="ExternalInput").ap()
    bT = nc.dram_tensor("bT", [128, 28], F32, kind="ExternalInput").ap()
    out = nc.dram_tensor("mod", [128, 56], F32, kind="ExternalOutput").ap()
    P = Prog(nc)
    c_sb = P.sbuf([128, 16], F32); rc = Res()
    ca = P.sbuf([128, 16], F32); rca = Res()
    b_sb = P.sbuf([128, 28], F32); rb = Res()
    wt = P.sbuf([128, 8, 3584], F32); rw = [Res() for _ in range(8)]
    o_sb = P.sbuf([128, 28, 2], F32); ro = Res()
    ps = P.psum([128, 28, 2], F32); rps = Res()
    P.dma("sp", c_sb[:], cT[:, :], writes=[rc])
    P.dma("sp", b_sb[:], bT[:, :], writes=[rb])
    for k in range(8):
        P.dma("sp", wt[:, k, :], W[k * 128:(k + 1) * 128, :], writes=[rw[k]])
    P.op("act", "activation", out=ca[:], in_=c_sb[:], func=AF.Silu, reads=[rc], writes=[rca])
    for j in range(28):
        for k in range(8):
            P.mm(ps[:, j, :], wt[:, k, j * 128:(j + 1) * 128], ca[:, 2 * k:2 * k + 2],
                 start=(k == 0), stop=(k == 7), reads=[rw[k], rca], writes=[rps])
    for b in range(2):
        P.op("dve", "tensor_tensor", out=o_sb[:, :, b], in0=ps[:, :, b], in1=b_sb[:, :], op=ALU.add,
             reads=[rps, rb], writes=[ro])
    P.dma("sp", out[:, :], o_sb[:].rearrange("p a b -> p (a b)"), reads=[ro], is_output=True)
    P.emit()
    return nc


class Ctx:
    pass


def setup_common(P, c):
    c.ones_bf = P.sbuf([128, 128], BF16)
    c.r_ones = Res()
    P.op("dve", "memset", c.ones_bf[:], 1.0, writes=[c.r_ones])
    c.eps_t = P.sbuf([128, 1], F32)
    c.r_eps = Res()
    P.op("dve", "memset", c.eps_t[:], EPS, writes=[c.r_eps])


def make_gs(P, c, vec, rvec, col_ng, col_sc):
    gs = P.sbuf([128, 8], F32)
    rg = Res()
    P.op("dve", "scalar_tensor_tensor", out=gs[:], in0=vec[:, col_sc:col_sc + 8], scalar=1.0,
         in1=vec[:, col_ng:col_ng + 8], op0=ALU.add, op1=ALU.mult, reads=[rvec], writes=[rg])
    return gs, rg


def emit_norm(P, c, pp, h, rh, toks, gs, rgs, sh_ap, rsh, out, rout, scr, otoks=None):
    if otoks is None:
        otoks = toks
    sq, rsq = scr["sq"]
    P.op("act", "activation", out=sq[:], in_=h[:, :, toks], func=AF.Square, reads=list(rh), writes=[rsq])
    ps, rps = pp.get()
    for k in range(8):
        P.mm(ps[:], c.ones_bf[:], sq[:, k, :], start=(k == 0), stop=(k == 7),
             reads=[c.r_ones, rsq], writes=[rps])
    ln, rln = scr["ln"]
    P.op("act", "activation", out=ln[:], in_=ps[:], func=AF.Ln, scale=1.0 / D, bias=c.eps_t[:],
         reads=[rps, c.r_eps], writes=[rln])
    rstd, rrstd = scr["rstd"]
    P.op("act", "activation", out=rstd[:], in_=ln[:], func=AF.Exp, scale=-0.5, reads=[rln], writes=[rrstd])
    for k in range(8):
        u, ru = scr["u"][k % 2]
        P.op("dve", "tensor_tensor", out=u[:], in0=h[:, k, toks], in1=rstd[:], op=ALU.mult,
             reads=[rh[k], rrstd], writes=[ru])
        P.op("act", "activation", out=out[:, k, otoks], in_=u[:], func=AF.Identity,
             scale=gs[:, k:k + 1], bias=sh_ap(k), reads=[ru, rgs, rsh], writes=[rout[k]])


def make_scr(P):
    return {
        "sq": (P.sbuf([128, 8, TT], BF16), Res()),
        "ln": (P.sbuf([128, TT], F32), Res()),
        "rstd": (P.sbuf([128, TT], F32), Res()),
        "u": [(P.sbuf([128, TT], F32), Res()) for _ in range(2)],
    }


def build_norm0():
    nc = bass.Bass("TRN2", target_bir_lowering=False)
    hT = nc.dram_tensor("hT", [D, TC], F32, kind="ExternalInput").ap()
    vecd = nc.dram_tensor("vec", [128, 24], F32, kind="ExternalInput").ap()
    aT = nc.dram_tensor("aT", [D, TC], BF16, kind="ExternalOutput").ap()
    hTv = hT.rearrange("(k p) t -> p k t", p=128)
    aTv = aT.rearrange("(k p) t -> p k t", p=128)
    P = Prog(nc)
    c = Ctx()
    setup_common(P, c)
    pp = PsumPool(P, 4)
    vec = P.sbuf([128, 24], F32); rvec = Res()
    P.dma("sp", vec[:], vecd[:, :], writes=[rvec])
    gs, rgs = make_gs(P, c, vec, rvec, 0, 8)
    scr = make_scr(P)
    hb = [(P.sbuf([128, 8, TT], F32), [Res() for _ in range(8)]) for _ in range(2)]
    ab = [(P.sbuf([128, 8, TT], BF16), [Res() for _ in range(8)]) for _ in range(2)]
    for t in range(TC // TT):
        h, rh = hb[t % 2]
        a, ra = ab[t % 2]
        P.dma("sp", h[:], hTv[:, :, t * TT:(t + 1) * TT], writes=rh)
        emit_norm(P, c, pp, h, rh, slice(0, TT), gs, rgs, lambda k: vec[:, 16 + k:17 + k], rvec, a, ra, scr)
        P.dma("sp", aTv[:, :, t * TT:(t + 1) * TT], a[:], reads=ra, is_output=True)
    P.emit()
    return nc


def build_post(KC, moe, final, extra_kv, FF, NFC):
    E = 8 if moe else 1
    NP = 4
    PT = TC // NP
    NTT = PT // TT
    NFG = FF // (NFC * 128)
    GW = NFC * 128
    nc = bass.Bass("TRN2", target_bir_lowering=False)
    hT = nc.dram_tensor("hT", [D, TC], F32, kind="ExternalInput").ap()
    mixT = nc.dram_tensor("mixT", [KC * 128, TC], BF16, kind="ExternalInput").ap()
    wo = nc.dram_tensor("wo", [KC * 128, D], F32, kind="ExternalInput").ap()
    wg = nc.dram_tensor("wg", [E, D, FF], F32, kind="ExternalInput").ap()
    wu = nc.dram_tensor("wu", [E, D, FF], F32, kind="ExternalInput").ap()
    wd = nc.dram_tensor("wd", [E, FF, D], F32, kind="ExternalInput").ap()
    vecd = nc.dram_tensor("vec", [128, 88], F32, kind="ExternalInput").ap()
    if moe:
        rwd = nc.dram_tensor("rw", [D, 8], F32, kind="ExternalInput").ap()
        rbd = nc.dram_tensor("rb", [128, 8], F32, kind="ExternalInput").ap()
        seld = nc.dram_tensor("sel", [8, 8 * 128], F32, kind="ExternalInput").ap()
        identd = nc.dram_tensor("ident", [128, 128], F32, kind="ExternalInput").ap()
    if final:
        outT = nc.dram_tensor("outT", [D, TC], F32, kind="ExternalOutput").ap()
    else:
        houtT = nc.dram_tensor("houtT", [D, TC], F32, kind="ExternalOutput").ap()
        anT = nc.dram_tensor("anT", [D, TC], BF16, kind="ExternalOutput").ap()
    if extra_kv:
        kvT = nc.dram_tensor("kvT", [D, TC], BF16, kind="ExternalOutput").ap()
    fm = lambda ap: ap.rearrange("(k p) t -> p k t", p=128)
    hTv, mixTv = fm(hT), fm(mixT)

    P = Prog(nc)
    c = Ctx()
    setup_common(P, c)
    pp = PsumPool(P, 7)
    vec = P.sbuf([128, 88], F32); rvec = Res()
    P.dma("sp", vec[:], vecd[:, :], writes=[rvec])
    col = lambda g, k: vec[:, g * 8 + k:g * 8 + k + 1]
    gs2, rgs2 = make_gs(P, c, vec, rvec, 8, 16)
    gsN, rgsN = make_gs(P, c, vec, rvec, 40, 48)
    if extra_kv:
        gsK, rgsK = make_gs(P, c, vec, rvec, 64, 72)
    scr = make_scr(P)

    wo_sb = P.sbuf([128, KC, D], BF16); rwo = Res()
    P.dma("pool", wo_sb[:], wo.rearrange("(k p) d -> p k d", p=128), writes=[rwo])

    if moe:
        rw_sb = P.sbuf([128, 8, 8], BF16); rrw = Res()
        P.dma("pool", rw_sb[:], rwd.rearrange("(k p) e -> p k e", p=128), writes=[rrw])
        rb_sb = P.sbuf([128, 8], F32); rrb = Res()
        P.dma("sp", rb_sb[:], rbd[:, :], writes=[rrb])
        sel_sb = P.sbuf([8, 8 * 128], F32); rsel = Res()
        P.dma("sp", sel_sb[:], seld[:, :], writes=[rsel])
        ident = P.sbuf([128, 128], F32); rid = Res()
        P.dma("sp", ident[:], identd[:, :], writes=[rid])
        gT = P.sbuf([8, PT], F32); rgT = Res()
        gb = [(P.sbuf([128, PT], BF16), Res()) for _ in range(2)]
        small = P.psum([128, 512], F32); rsmall = Res()
        _st = {}

        def stile(name, shape):
            if name not in _st:
                _st[name] = (P.sbuf(shape, F32), Res())
            return _st[name]

    hbuf = P.sbuf([128, 8, PT], F32)
    rh = [[Res() for _ in range(8)] for _ in range(NTT)]
    mbuf = P.sbuf([128, 8, PT], BF16)
    rm = [[Res() for _ in range(8)] for _ in range(NTT)]
    mixb = (P.sbuf([128, KC, TT], BF16), Res())
    wgb = [(P.sbuf([128, 8, GW], BF16), Res()) for _ in range(2)]
    wub = [(P.sbuf([128, 8, GW], BF16), Res()) for _ in range(2)]
    wdb = [(P.sbuf([128, NFC, D], BF16), Res()) for _ in range(2)]
    actb = [(P.sbuf([128, NFC, TT], BF16), [Res() for _ in range(NFC)]) for _ in range(2)]
    sil = [(P.sbuf([128, TT], BF16), Res()) for _ in range(3)]
    tmpb = [(P.sbuf([128, TT], BF16), Res()) for _ in range(2)]
    outb = [(P.sbuf([128, 8, TT], F32 if final else BF16), [Res() for _ in range(8)]) for _ in range(2)]
    nsil = [0]
    nob = [0]

    for ps_i in range(NP):
        t0 = ps_i * PT
        for tt in range(NTT):
            toks = slice(tt * TT, (tt + 1) * TT)
            g0 = t0 + tt * TT
            P.dma("sp", hbuf[:, :, toks], hTv[:, :, g0:g0 + TT], writes=rh[tt])
            mx, rmx = mixb
            P.dma("sp", mx[:], mixTv[:, :, g0:g0 + TT], writes=[rmx])
            for d in range(8):
                ps, rps = pp.get()
                for k in range(KC):
                    P.mm(ps[:], wo_sb[:, k, d * 128:(d + 1) * 128], mx[:, k, :], start=(k == 0),
                         stop=(k == KC - 1), reads=[rwo, rmx], writes=[rps])
                P.op("dve", "scalar_tensor_tensor", out=hbuf[:, d, toks], in0=ps[:], scalar=col(0, d),
                     in1=hbuf[:, d, toks], op0=ALU.mult, op1=ALU.add,
                     reads=[rps, rvec, rh[tt][d]], writes=[rh[tt][d]])
            emit_norm(P, c, pp, hbuf, rh[tt], toks, gs2, rgs2, lambda k: col(3, k), rvec, mbuf, rm[tt], scr)
            if moe:
                for sb in range(TT // 128):
                    tk = slice(tt * TT + sb * 128, tt * TT + (sb + 1) * 128)
                    for k in range(8):
                        P.mm(small[:, 0:8], mbuf[:, k, tk], rw_sb[:, k, :], start=(k == 0), stop=(k == 7),
                             reads=[rm[tt][k], rrw], writes=[rsmall])
                    lg, rlg = stile("lg", [128, 8])
                    P.op("dve", "tensor_tensor", out=lg[:], in0=small[:, 0:8], in1=rb_sb[:], op=ALU.add,
                         reads=[rsmall, rrb], writes=[rlg])
                    m1, rm1 = stile("m1", [128, 1])
                    P.op("dve", "reduce_max", out=m1[:], in_=lg[:], axis=AX.X, reads=[rlg], writes=[rm1])
                    k1, rk1 = stile("k1", [128, 8])
                    P.op("dve", "tensor_scalar", out=k1[:], in0=lg[:], scalar1=m1[:, 0:1], scalar2=None,
                         op0=ALU.is_equal, reads=[rlg, rm1], writes=[rk1])
                    lg2, rlg2 = stile("lg2", [128, 8])
                    P.op("dve", "scalar_tensor_tensor", out=lg2[:], in0=k1[:], scalar=-1e30, in1=lg[:],
                         op0=ALU.mult, op1=ALU.add, reads=[rk1, rlg], writes=[rlg2])
                    m2, rm2 = stile("m2", [128, 1])
                    P.op("dve", "reduce_max", out=m2[:], in_=lg2[:], axis=AX.X, reads=[rlg2], writes=[rm2])
                    k2, rk2 = stile("k2", [128, 8])
                    P.op("dve", "tensor_scalar", out=k2[:], in0=lg2[:], scalar1=m2[:, 0:1], scalar2=None,
                         op0=ALU.is_equal, reads=[rlg2, rm2], writes=[rk2])
                    dd, rdd = stile("dd", [128, 1])
                    P.op("dve", "tensor_tensor", out=dd[:], in0=m2[:], in1=m1[:], op=ALU.subtract,
                         reads=[rm1, rm2], writes=[rdd])
                    ee, ree = stile("ee", [128, 1])
                    P.op("act", "activation", out=ee[:], in_=dd[:], func=AF.Exp, reads=[rdd], writes=[ree])
                    den, rden = stile("den", [128, 1])
                    P.op("dve", "tensor_scalar", out=den[:], in0=ee[:], scalar1=1.0, scalar2=None, op0=ALU.add,
                         reads=[ree], writes=[rden])
                    w1, rw1 = stile("w1", [128, 1])
                    P.op("dve", "reciprocal", out=w1[:], in_=den[:], reads=[rden], writes=[rw1])
                    w2, rw2 = stile("w2", [128, 1])
                    P.op("dve", "tensor_tensor", out=w2[:], in0=ee[:], in1=w1[:], op=ALU.mult,
                         reads=[ree, rw1], writes=[rw2])
                    t1, rt1 = stile("t1", [128, 8])
                    P.op("dve", "tensor_scalar", out=t1[:], in0=k1[:], scalar1=w1[:, 0:1], scalar2=None,
                         op0=ALU.mult, reads=[rk1, rw1], writes=[rt1])
                    gt, rgt = stile("gt", [128, 8])
                    P.op("dve", "scalar_tensor_tensor", out=gt[:], in0=k2[:], scalar=w2[:, 0:1], in1=t1[:],
                         op0=ALU.mult, op1=ALU.add, reads=[rk2, rw2, rt1], writes=[rgt])
                    P.op("pe", "transpose", small[0:8, 128:256], gt[:], ident[:], reads=[rgt, rid],
                         writes=[rsmall])
                    P.op("dve", "tensor_copy", out=gT[:, tk], in_=small[0:8, 128:256], reads=[rsmall],
                         writes=[rgT])
        steps = [(e, fg, tt) for e in range(E) for fg in range(NFG) for tt in range(NTT)]
        pend = None
        wslot = [0]

        def load_w(e, fg):
            i = wslot[0] % 2
            wslot[0] += 1
            g_, rg_ = wgb[i]; u_, ru_ = wub[i]; d_, rd_ = wdb[i]
            P.dma("pool", g_[:], wg[e, :, fg * GW:(fg + 1) * GW].rearrange("(k p) f -> p k f", p=128),
                  writes=[rg_])
            P.dma("pool", u_[:], wu[e, :, fg * GW:(fg + 1) * GW].rearrange("(k p) f -> p k f", p=128),
                  writes=[ru_])
            P.dma("pool", d_[:], wd[e, fg * GW:(fg + 1) * GW, :].rearrange("(c p) d -> p c d", p=128),
                  writes=[rd_])
            return i

        def down(e, fg, tt, wi, ai):
            toks = slice(tt * TT, (tt + 1) * TT)
            d_, rd_ = wdb[wi]
            a_, ra_ = actb[ai]
            for d in range(8):
                ps, rps = pp.get()
                for fc in range(NFC):
                    P.mm(ps[:], d_[:, fc, d * 128:(d + 1) * 128], a_[:, fc, :], start=(fc == 0),
                         stop=(fc == NFC - 1), reads=[rd_, ra_[fc]], writes=[rps])
                P.op("dve", "scalar_tensor_tensor", out=hbuf[:, d, toks], in0=ps[:], scalar=col(4, d),
                     in1=hbuf[:, d, toks], op0=ALU.mult, op1=ALU.add,
                     reads=[rps, rvec, rh[tt][d]], writes=[rh[tt][d]])

        cur_w = None
        cur_key = None
        astep = 0
        cur_gb = None
        for (e, fg, tt) in steps:
            toks = slice(tt * TT, (tt + 1) * TT)
            if moe and fg == 0 and tt == 0:
                gbt, rgb = gb[e % 2]
                for t2 in range(NTT):
                    tk2 = slice(t2 * TT, (t2 + 1) * TT)
                    ps, rps = pp.get()
                    P.mm(ps[:], sel_sb[:, e * 128:(e + 1) * 128], gT[:, tk2], start=True, stop=True,
                         reads=[rsel, rgT], writes=[rps])
                    P.op("act", "activation", out=gbt[:, tk2], in_=ps[:], func=AF.Identity,
                         reads=[rps], writes=[rgb])
                cur_gb = (gbt, rgb)
            if cur_key != (e, fg):
                cur_w = load_w(e, fg)
                cur_key = (e, fg)
            g_, rg_ = wgb[cur_w]; u_, ru_ = wub[cur_w]
            ai = astep % 2
            astep += 1
            a_, ra_ = actb[ai]
            for fc in range(NFC):
                psg, rpsg = pp.get()
                for k in range(8):
                    P.mm(psg[:], g_[:, k, fc * 128:(fc + 1) * 128], mbuf[:, k, toks], start=(k == 0),
                         stop=(k == 7), reads=[rg_, rm[tt][k]], writes=[rpsg])
                psu, rpsu = pp.get()
                for k in range(8):
                    P.mm(psu[:], u_[:, k, fc * 128:(fc + 1) * 128], mbuf[:, k, toks], start=(k == 0),
                         stop=(k == 7), reads=[ru_, rm[tt][k]], writes=[rpsu])
                s_, rs_ = sil[nsil[0] % 3]
                nsil[0] += 1
                P.op("act", "activation", out=s_[:], in_=psg[:], func=AF.Silu, reads=[rpsg], writes=[rs_])
                if moe:
                    t_, rt_ = tmpb[fc % 2]
                    P.op("dve", "tensor_tensor", out=t_[:], in0=psu[:], in1=s_[:], op=ALU.mult,
                         reads=[rpsu, rs_], writes=[rt_])
                    P.op("dve", "tensor_tensor", out=a_[:, fc, :], in0=t_[:], in1=cur_gb[0][:, toks],
                         op=ALU.mult, reads=[rt_, cur_gb[1]], writes=[ra_[fc]])
                else:
                    P.op("dve", "tensor_tensor", out=a_[:, fc, :], in0=psu[:], in1=s_[:], op=ALU.mult,
                         reads=[rpsu, rs_], writes=[ra_[fc]])
            if pend is not None:
                down(*pend)
            pend = (e, fg, tt, cur_w, ai)
        down(*pend)
        for tt in range(NTT):
            toks = slice(tt * TT, (tt + 1) * TT)
            g0 = t0 + tt * TT
            if not final:
                P.dma("sp", fm(houtT)[:, :, g0:g0 + TT], hbuf[:, :, toks], reads=rh[tt], is_output=True)
            o_, ro_ = outb[nob[0] % 2]; nob[0] += 1
            emit_norm(P, c, pp, hbuf, rh[tt], toks, gsN, rgsN, lambda k: col(7, k), rvec, o_, ro_, scr,
                      otoks=slice(0, TT))
            P.dma("sp", fm(outT if final else anT)[:, :, g0:g0 + TT], o_[:], reads=ro_, is_output=True)
            if extra_kv:
                o_, ro_ = outb[nob[0] % 2]; nob[0] += 1
                emit_norm(P, c, pp, hbuf, rh[tt], toks, gsK, rgsK, lambda k: col(10, k), rvec, o_, ro_, scr,
                          otoks=slice(0, TT))
                P.dma("sp", fm(kvT)[:, :, g0:g0 + TT], o_[:], reads=ro_, is_output=True)
    P.emit()
    return nc


S_LEN = 16384


def build_ret(NTILES=S_LEN // TT):
    nc = bass.Bass("TRN2", target_bir_lowering=False)
    S = NTILES * TT
    aT = nc.dram_tensor("aT", [D, S], BF16, kind="ExternalInput").ap()
    win = nc.dram_tensor("win", [D, 1536], F32, kind="ExternalInput").ap()
    cosd = nc.dram_tensor("cosT", [128, S], F32, kind="ExternalInput").ap()
    sind = nc.dram_tensor("sinT", [128, S], F32, kind="ExternalInput").ap()
    maskd = nc.dram_tensor("maskbd", [128, 128], F32, kind="ExternalInput").ap()
    cvd = nc.dram_tensor("cvec", [128, 8], F32, kind="ExternalInput").ap()
    identd = nc.dram_tensor("identb", [128, 128], BF16, kind="ExternalInput").ap()
    ygT = nc.dram_tensor("ygT", [512, S], BF16, kind="ExternalOutput").ap()
    aTv = aT.rearrange("(k p) t -> p k t", p=128)
    ygTv = ygT.rearrange("(j p) t -> p j t", p=128)

    P = Prog(nc)
    pp = PsumPool(P, 4)
    w_sb = P.sbuf([128, 8, 1536], BF16); rw = Res()
    P.dma("pool", w_sb[:], win.rearrange("(k p) f -> p k f", p=128), writes=[rw])
    mask = P.sbuf([128, 128], F32); rmask = Res()
    P.dma("sp", mask[:], maskd[:, :], writes=[rmask])
    cv = P.sbuf([128, 8], F32); rcv = Res()
    P.dma("sp", cv[:], cvd[:, :], writes=[rcv])
    ident = P.sbuf([128, 128], BF16); rid = Res()
    P.dma("sp", ident[:], identd[:, :], writes=[rid])
    KD, QD, QD2, CDEC, NH = [cv[:, i:i + 1] for i in range(5)]

    ab = [(P.sbuf([128, 8, TT], BF16), Res()) for _ in range(2)]
    cosb = [(P.sbuf([128, TT], F32), Res()) for _ in range(2)]
    sinb = [(P.sbuf([128, TT], F32), Res()) for _ in range(2)]
    qkf = [(P.sbuf([128, TT], F32), Res()) for _ in range(4)]
    rt = [(P.sbuf([128, TT], F32), Res()) for _ in range(4)]
    qTb = [(P.sbuf([128, 2, TT], BF16), [Res(), Res()]) for _ in range(2)]
    kTb = [(P.sbuf([128, 2, TT], BF16), [Res(), Res()]) for _ in range(2)]
    vb = [(P.sbuf([128, 4, 512], BF16), [Res() for _ in range(4)]) for _ in range(2)]
    gb = [(P.sbuf([128, 4, 512], BF16), [Res() for _ in range(4)]) for _ in range(2)]
    ktm = [(P.sbuf([128, 4, 256], BF16), [Res() for _ in range(4)]) for _ in range(2)]
    pTb = [(P.sbuf([128, 128], BF16), Res()) for _ in range(2)]
    st = P.sbuf([128, 2, 512], F32); rst = [Res(), Res()]
    stb = [(P.sbuf([128, 2, 512], BF16), Res()) for _ in range(2)]
    yb = [(P.sbuf([128, 512], BF16), Res()) for _ in range(2)]
    ygb = [(P.sbuf([128, 512], BF16), Res()) for _ in range(2)]
    ygTb = [(P.sbuf([128, 4, TT], BF16), [Res() for _ in range(4)]) for _ in range(2)]
    stats = P.sbuf([128, 6], F32); rstats = Res()
    mv = P.sbuf([128, 2], F32); rmv = Res()
    vv = P.sbuf([128, 1], F32); rvv = Res()
    rs = P.sbuf([128, 1], F32); rrs = Res()
    scl = P.sbuf([128, 1], F32); rscl = Res()
    ps_kv = [(P.psum([128, 512], F32), Res()) for _ in range(2)]
    ps_s = P.psum([128, 512], F32); rps_s = Res()
    ps_t = P.psum([128, 1024], BF16); rps_t = [Res(), Res()]

    P.op("dve", "memset", st[:], 0.0, writes=rst)
    P.op("dve", "memset", stb[1][0][:], 0.0, writes=[stb[1][1]])
    nstb = [1]

    for t in range(NTILES):
        a, ra = ab[t % 2]
        co, rco = cosb[t % 2]
        si, rsi = sinb[t % 2]
        P.dma("sp", a[:], aTv[:, :, t * TT:(t + 1) * TT], writes=[ra])
        P.dma("sp", co[:], cosd[:, t * TT:(t + 1) * TT], writes=[rco])
        P.dma("sp", si[:], sind[:, t * TT:(t + 1) * TT], writes=[rsi])
        for j in range(4):
            ps, rps = pp.get()
            for k in range(8):
                P.mm(ps[:], w_sb[:, k, j * 128:(j + 1) * 128], a[:, k, :], start=(k == 0), stop=(k == 7),
                     reads=[rw, ra], writes=[rps])
            f, rf = qkf[j]
            P.op("act", "activation", out=f[:], in_=ps[:], func=AF.Identity, reads=[rps], writes=[rf])
        qT, rqT = qTb[t % 2]
        kT, rkT = kTb[t % 2]
        for (eng, A, B, dst, rdst, tmp) in (("dve", qkf[0], qkf[1], qT, rqT, rt[0:2]),
                                            ("pool", qkf[2], qkf[3], kT, rkT, rt[2:4])):
            (fa, rfa), (fb, rfb) = A, B
            (t1, rt1), (t2, rt2) = tmp
            P.op(eng, "tensor_tensor", out=t1[:], in0=fa[:], in1=co[:], op=ALU.mult, reads=[rfa, rco], writes=[rt1])
            P.op(eng, "tensor_tensor", out=t2[:], in0=fb[:], in1=si[:], op=ALU.mult, reads=[rfb, rsi], writes=[rt2])
            P.op(eng, "tensor_tensor", out=dst[:, 0, :], in0=t1[:], in1=t2[:], op=ALU.subtract,
                 reads=[rt1, rt2], writes=[rdst[0]])
            P.op(eng, "tensor_tensor", out=t1[:], in0=fb[:], in1=co[:], op=ALU.mult, reads=[rfb, rco], writes=[rt1])
            P.op(eng, "tensor_tensor", out=t2[:], in0=fa[:], in1=si[:], op=ALU.mult, reads=[rfa, rsi], writes=[rt2])
            P.op(eng, "tensor_tensor", out=dst[:, 1, :], in0=t1[:], in1=t2[:], op=ALU.add,
                 reads=[rt1, rt2], writes=[rdst[1]])
        v, rv = vb[t % 2]
        g, rg = gb[t % 2]
        for blk in range(4):
            bs = slice(blk * 128, (blk + 1) * 128)
            ps, rps = pp.get()
            for k in range(8):
                P.mm(ps[:], a[:, k, bs], w_sb[:, k, 512:1024], start=(k == 0), stop=(k == 7),
                     reads=[rw, ra], writes=[rps])
            P.op("act", "activation", out=v[:, blk, :], in_=ps[:], func=AF.Identity, reads=[rps], writes=[rv[blk]])
            ps, rps = pp.get()
            for k in range(8):
                P.mm(ps[:], a[:, k, bs], w_sb[:, k, 1024:1536], start=(k == 0), stop=(k == 7),
                     reads=[rw, ra], writes=[rps])
            P.op("act", "activation", out=g[:, blk, :], in_=ps[:], func=AF.Silu, reads=[rps], writes=[rg[blk]])
        km, rkm = ktm[t % 2]
        for blk in range(4):
            bs = slice(blk * 128, (blk + 1) * 128)
            for kc in range(2):
                P.op("pe", "transpose", ps_t[:, kc * 128:(kc + 1) * 128], kT[:, kc, bs], ident[:],
                     reads=[rkT[kc], rid], writes=[rps_t[0]])
            P.op("act", "activation", out=km[:, blk, :], in_=ps_t[:, 0:256], func=AF.Identity, scale=KD,
                 reads=[rps_t[0], rcv], writes=[rkm[blk]])
        ygT_sb, rygT = ygTb[t % 2]
        for blk in range(4):
            bs = slice(blk * 128, (blk + 1) * 128)
            for kc in range(2):
                P.mm(ps_s[:, 0:128], kT[:, kc, bs], qT[:, kc, bs], start=(kc == 0), stop=(kc == 1),
                     reads=[rkT[kc], rqT[kc]], writes=[rps_s])
            pT, rpT = pTb[blk % 2]
            P.op("dve", "tensor_tensor", out=pT[:], in0=ps_s[:, 0:128], in1=mask[:], op=ALU.mult,
                 reads=[rps_s, rmask], writes=[rpT])
            pso, rpso = pp.get()
            P.mm(pso[:], pT[:], v[:, blk, :], start=True, stop=False, reads=[rpT, rv[blk]], writes=[rpso])
            for half in range(2):
                o = half * 64
                cs = slice(blk * 128 + o, blk * 128 + o + 64)
                sb_, rsb_ = stb[nstb[0]]
                for kc in range(2):
                    P.mm(pso[o:o + 64, :], qT[:, kc, cs], sb_[:, kc, :], start=False,
                         stop=(kc == 1 and half == 1), reads=[rqT[kc], rsb_], writes=[rpso])
                for kc in range(2):
                    pk, rpk = ps_kv[kc]
                    P.mm(pk[:], km[o:o + 64, blk, kc * 128:(kc + 1) * 128], v[o:o + 64, blk, :], start=True,
                         stop=True, reads=[rkm[blk], rv[blk]], writes=[rpk])
                    P.op("dve", "scalar_tensor_tensor", out=st[:, kc, :], in0=st[:, kc, :], scalar=CDEC,
                         in1=pk[:], op0=ALU.mult, op1=ALU.add, reads=[rst[kc], rcv, rpk], writes=[rst[kc]])
                nstb[0] ^= 1
                sb2, rsb2 = stb[nstb[0]]
                P.op("act", "activation", out=sb2[:], in_=st[:], func=AF.Identity, reads=rst, writes=[rsb2])
            P.op("dve", "bn_stats", out=stats[:], in_=pso[:], reads=[rpso], writes=[rstats])
            P.op("dve", "bn_aggr", out=mv[:], in_=stats[:], reads=[rstats], writes=[rmv])
            P.op("dve", "tensor_scalar", out=vv[:], in0=mv[:, 1:2], scalar1=QD2, scalar2=EPS, op0=ALU.mult,
                 op1=ALU.add, reads=[rmv, rcv], writes=[rvv])
            P.op("pool", "tensor_tensor", out=rs[:], in0=vv[:], in1=NH, op=ALU.pow, reads=[rvv, rcv],
                 writes=[rrs])
            P.op("pool", "tensor_tensor", out=scl[:], in0=rs[:], in1=QD, op=ALU.mult, reads=[rrs, rcv],
                 writes=[rscl])
            y, ry = yb[blk % 2]
            P.op("dve", "tensor_scalar", out=y[:], in0=pso[:], scalar1=mv[:, 0:1], scalar2=scl[:, 0:1],
                 op0=ALU.subtract, op1=ALU.mult, reads=[rpso, rmv, rscl], writes=[ry])
            yg, ryg = ygb[blk % 2]
            P.op("pool", "tensor_tensor", out=yg[:], in0=y[:], in1=g[:, blk, :], op=ALU.mult,
                 reads=[ry, rg[blk]], writes=[ryg])
            for j in range(4):
                P.op("pe", "transpose", ps_t[:, 512 + j * 128:512 + (j + 1) * 128], yg[:, j * 128:(j + 1) * 128],
                     ident[:], reads=[ryg, rid], writes=[rps_t[1]])
            P.op("act", "activation", out=ygT_sb[:, :, bs],
                 in_=ps_t[:, 512:1024].rearrange("p (j t) -> p j t", j=4), func=AF.Identity,
                 reads=[rps_t[1]], writes=[rygT[blk]])
        P.dma("sp", ygTv[:, :, t * TT:(t + 1) * TT], ygT_sb[:], reads=rygT, is_output=True)
    P.emit()
    return nc


def build_attn(NQT=S_LEN // TT, stage=9, NH=4):
    nc = bass.Bass("TRN2", target_bir_lowering=False)
    S = NQT * TT
    NB = S // 128
    aT = nc.dram_tensor("aT", [D, S], BF16, kind="ExternalInput").ap()
    kvT = nc.dram_tensor("kvT", [D, S], BF16, kind="ExternalInput").ap()
    wqd = nc.dram_tensor("wq", [D, 268], F32, kind="ExternalInput").ap()
    wgd = nc.dram_tensor("wgt", [D, 256], F32, kind="ExternalInput").ap()
    wkd = nc.dram_tensor("wk", [D, 256], F32, kind="ExternalInput").ap()
    wvfd = nc.dram_tensor("wvf", [D, 260], F32, kind="ExternalInput").ap()
    bfd = nc.dram_tensor("bf", [128, 4], F32, kind="ExternalInput").ap()
    trid = nc.dram_tensor("tri", [128, 128], F32, kind="ExternalInput").ap()
    tmd = nc.dram_tensor("trimask", [128, 128], BF16, kind="ExternalInput").ap()
    idbd = nc.dram_tensor("identb", [128, 128], BF16, kind="ExternalInput").ap()
    shd = nc.dram_tensor("shiftsel", [128, 64], F32, kind="ExternalInput").ap()
    ogT = nc.dram_tensor("ogT", [256, S], BF16, kind="ExternalOutput").ap()
    fm = lambda ap: ap.rearrange("(k p) t -> p k t", p=128)
    aTv, kvTv = fm(aT), fm(kvT)

    P = Prog(nc)
    ppS = PsumPool(P, 3)
    ppB = PsumPool(P, 3)
    ps_o = [(P.psum([128, 512], F32), Res()) for _ in range(2)]

    def wload(dram, n):
        t_ = P.sbuf([128, 8, n], BF16); r_ = Res()
        P.dma("pool", t_[:], dram.rearrange("(k p) f -> p k f", p=128), writes=[r_])
        return t_, r_
    wq, rwq = wload(wqd, 268)
    wg, rwg = wload(wgd, 256)
    wk, rwk = wload(wkd, 256)
    wvf, rwvf = wload(wvfd, 260)
    bft = P.sbuf([128, 4], F32); rbf = Res()
    P.dma("sp", bft[:], bfd[:, :], writes=[rbf])
    negb = P.sbuf([128, 4], F32); rnegb = Res()
    P.op("dve", "tensor_scalar", out=negb[:], in0=bft[:], scalar1=-1.0, scalar2=None, op0=ALU.mult,
         reads=[rbf], writes=[rnegb])
    tri = P.sbuf([128, 128], F32); rtri = Res()
    P.dma("sp", tri[:], trid[:, :], writes=[rtri])
    trimask = P.sbuf([128, 128], BF16); rtm = Res()
    P.dma("sp", trimask[:], tmd[:, :], writes=[rtm])
    identb = P.sbuf([128, 128], BF16); ridb = Res()
    P.dma("sp", identb[:], idbd[:, :], writes=[ridb])
    shiftsel = P.sbuf([128, 64], F32); rshs = Res()
    P.dma("sp", shiftsel[:], shd[:, :], writes=[rshs])
    onesf = P.sbuf([128, 128], F32); ronesf = Res()
    P.op("dve", "memset", onesf[:], 1.0, writes=[ronesf])

    K_aug = P.sbuf([67, S], BF16); rK = [Res() for _ in range(NQT)]
    rKones = Res()
    P.op("dve", "memset", K_aug[:], 8.0, writes=[rKones])
    V_aug = P.sbuf([128, NB, 128], BF16); rV = [Res() for _ in range(NB)]
    rVones = Res()
    P.op("dve", "memset", V_aug[:], 1.0, writes=[rVones])
    e_all = P.sbuf([128, NB], F32); re_all = Res()
    sp_all = P.sbuf([128, NB], F32); rsp = Res()
    tot = P.sbuf([128, NB], F32); rtot = Res()
    cs = P.sbuf([128, NB], F32); rcs = Res()
    tmpF = P.sbuf([128, NB], F32); rtmpF = Res()
    negF = P.sbuf([128, NB], F32); rnegF = Res()
    Pc = P.sbuf([128, NB, 67], BF16); rPc = Res()
    P.op("dve", "memset", Pc[:], 0.0, writes=[rPc])
    r1 = P.sbuf([128, NB], F32); rr1 = Res()
    r2 = P.sbuf([128, NB], F32); rr2 = Res()
    onesNB = P.sbuf([128, NB], F32); ronesNB = Res()
    P.op("dve", "memset", onesNB[:], 1.0, writes=[ronesNB])

    kvb = [(P.sbuf([128, 8, TT], BF16), Res()) for _ in range(2)]
    ab = [(P.sbuf([128, 8, TT], BF16), Res()) for _ in range(2)]
    Qb = [(P.sbuf([67, TT], BF16), Res()) for _ in range(2)]
    pTb = [(P.sbuf([128, TT], BF16), Res()) for _ in range(3)]
    eg = P.sbuf([64, TT], F32); reg = Res()
    sgb = [(P.sbuf([64, TT], F32), Res()) for _ in range(2)]
    lr = P.sbuf([128, TT], F32); rlr = Res()
    rl = P.sbuf([64, TT], F32); rrl = Res()
    t64 = P.sbuf([64, TT], F32); rt64 = Res()
    ogb = [(P.sbuf([64, TT], BF16), Res()) for _ in range(2)]
    nkv = [0]
    na = [0]
    npt = [0]

    for h in range(NH):
        hs = slice(h * 64, (h + 1) * 64)
        for tt in range(NQT):
            kv, rkv = kvb[nkv[0] % 2]; nkv[0] += 1
            P.dma("sp", kv[:], kvTv[:, :, tt * TT:(tt + 1) * TT], writes=[rkv])
            ps, rps = ppB.get()
            for k in range(8):
                P.mm(ps[0:64, :], wk[:, k, hs], kv[:, k, :], start=(k == 0), stop=(k == 7),
                     reads=[rwk, rkv], writes=[rps])
            P.op("act", "activation", out=K_aug[0:64, tt * TT:(tt + 1) * TT], in_=ps[0:64, :], func=AF.Identity,
                 reads=[rps, rKones], writes=[rK[tt]])
            for blk in range(4):
                gb_ = tt * 4 + blk
                bs = slice(blk * 128, (blk + 1) * 128)
                ps, rps = ppB.get()
                for k in range(8):
                    P.mm(ps[:, 0:65], kv[:, k, bs], wvf[:, k, h * 65:(h + 1) * 65], start=(k == 0), stop=(k == 7),
                         reads=[rwvf, rkv], writes=[rps])
                P.op("act", "activation", out=V_aug[:, gb_, 0:64], in_=ps[:, 0:64], func=AF.Identity,
                     reads=[rps, rVones], writes=[rV[gb_]])
                P.op("act", "activation", out=e_all[:, gb_:gb_ + 1], in_=ps[:, 64:65], func=AF.Exp, scale=-1.0,
                     bias=negb[:, h:h + 1], reads=[rps, rnegb], writes=[re_all])
        P.op("act", "activation", out=sp_all[:], in_=e_all[:], func=AF.Ln, bias=onesf[:, 0:1],
             reads=[re_all, ronesf], writes=[rsp])
        psF, rpsF = ppB.get()
        P.mm(psF[:, 0:NB], tri[:], sp_all[:], start=True, stop=True, reads=[rtri, rsp], writes=[rpsF])
        psT, rpsT = ppB.get()
        P.mm(psT[:, 0:NB], onesf[:], sp_all[:], start=True, stop=True, reads=[ronesf, rsp], writes=[rpsT])
        P.op("dve", "tensor_copy", out=tot[:], in_=psT[:, 0:NB], reads=[rpsT], writes=[rtot])
        P.op("dve", "tensor_tensor_scan", out=cs[:], data0=onesNB[:], data1=tot[:], initial=0.0,
             op0=ALU.mult, op1=ALU.add, reads=[ronesNB, rtot], writes=[rcs])
        P.op("dve", "tensor_tensor", out=tmpF[:], in0=cs[:], in1=tot[:], op=ALU.subtract,
             reads=[rcs, rtot], writes=[rtmpF])
        P.op("dve", "tensor_tensor", out=negF[:], in0=psF[:, 0:NB], in1=tmpF[:], op=ALU.add,
             reads=[rpsF, rtmpF], writes=[rnegF])
        P.op("dve", "tensor_scalar", out=Pc[:, :, 64], in0=negF[:], scalar1=-1.0, scalar2=None, op0=ALU.mult,
             reads=[rnegF], writes=[rPc])
        P.op("dve", "scalar_tensor_tensor", out=r1[:], in0=negF[:], scalar=-1.0, in1=Pc[:, :, 64], op0=ALU.mult,
             op1=ALU.subtract, reads=[rnegF, rPc], writes=[rr1])
        P.op("dve", "tensor_copy", out=Pc[:, :, 65], in_=r1[:], reads=[rr1], writes=[rPc])
        P.op("dve", "tensor_tensor", out=r2[:], in0=r1[:], in1=Pc[:, :, 65], op=ALU.subtract,
             reads=[rr1, rPc], writes=[rr2])
        P.op("dve", "tensor_copy", out=Pc[:, :, 66], in_=r2[:], reads=[rr2], writes=[rPc])

        if stage == 1:
            og, rog = ogb[0]
            P.op("dve", "tensor_copy", out=og[:, 0:NB], in_=negF[0:64, :], reads=[rnegF, rPc], writes=[rog])
            P.dma("sp", ogT[h * 64:(h + 1) * 64, 0:TT], og[:], reads=[rog], is_output=True)
            continue
        for t in range(NQT):
            a, ra = ab[na[0] % 2]
            Q, rQ = Qb[na[0] % 2]
            sg, rsg = sgb[na[0] % 2]
            og, rog = ogb[na[0] % 2]
            pso, rpso = ps_o[na[0] % 2]
            na[0] += 1
            P.dma("sp", a[:], aTv[:, :, t * TT:(t + 1) * TT], writes=[ra])
            ps, rps = ppB.get()
            for k in range(8):
                P.mm(ps[0:67, :], wq[:, k, h * 67:(h + 1) * 67], a[:, k, :], start=(k == 0), stop=False,
                     reads=[rwq, ra], writes=[rps])
            for blk in range(4):
                P.mm(ps[0:67, blk * 128:(blk + 1) * 128], Pc[:, t * 4 + blk, :], identb[:], start=False,
                     stop=(blk == 3), reads=[rPc, ridb], writes=[rps])
            P.op("act", "activation", out=Q[:], in_=ps[0:67, :], func=AF.Identity, reads=[rps], writes=[rQ])
            ps2, rps2 = ppB.get()
            for k in range(8):
                P.mm(ps2[0:64, :], wg[:, k, hs], a[:, k, :], start=(k == 0), stop=(k == 7),
                     reads=[rwg, ra], writes=[rps2])
            P.op("act", "activation", out=eg[:], in_=ps2[0:64, :], func=AF.Exp, scale=-1.0, reads=[rps2],
                 writes=[reg])
            P.op("dve", "tensor_scalar", out=eg[:], in0=eg[:], scalar1=1.0, scalar2=None, op0=ALU.add,
                 reads=[reg], writes=[reg])
            P.op("dve", "reciprocal", out=sg[:], in_=eg[:], reads=[reg], writes=[rsg])
            nblk = 4 * t + 4
            if stage == 2:
                P.op("dve", "tensor_copy", out=og[:], in_=Q[0:64, :], reads=[rQ, rsg], writes=[rog])
                P.dma("sp", ogT[h * 64:(h + 1) * 64, t * TT:(t + 1) * TT], og[:], reads=[rog], is_output=True)
                continue
            for j in range(nblk):
                o = j - 4 * t
                c0 = 128 * o if o > 0 else 0
                N = TT - c0
                pss, rpss = ppS.get()
                P.mm(pss[:, 0:N], K_aug[0:67, j * 128:(j + 1) * 128], Q[0:67, c0:TT], start=True, stop=True,
                     reads=[rK[j // 4], rKones, rQ], writes=[rpss])
                pT, rpT = pTb[npt[0] % 3]; npt[0] += 1
                P.op("act", "activation", out=pT[:, 0:N], in_=pss[:, 0:N], func=AF.Exp, scale=0.125,
                     bias=negF[:, j:j + 1], reads=[rpss, rnegF], writes=[rpT])
                if o >= 0:
                    P.op("pool", "tensor_tensor", out=pT[:, 0:128], in0=pT[:, 0:128], in1=trimask[:], op=ALU.mult,
                         reads=[rpT, rtm], writes=[rpT])
                P.mm(pso[:, c0:TT], V_aug[:, j, :], pT[:, 0:N], start=(j == 0), stop=(j == nblk - 1),
                     reads=[rV[j], rVones, rpT], writes=[rpso])
            if stage == 3:
                P.op("dve", "tensor_copy", out=og[:], in_=pso[0:64, :], reads=[rpso, rsg], writes=[rog])
                P.dma("sp", ogT[h * 64:(h + 1) * 64, t * TT:(t + 1) * TT], og[:], reads=[rog], is_output=True)
                continue
            P.op("act", "activation", out=lr[:], in_=pso[:], func=AF.Identity, reads=[rpso],
                 writes=[rlr])
            psb, rpsb = ppB.get()
            P.mm(psb[0:64, :], shiftsel[:], lr[:], start=True, stop=True, reads=[rshs, rlr],
                 writes=[rpsb])
            P.op("dve", "reciprocal", out=rl[:], in_=psb[0:64, :], reads=[rpsb], writes=[rrl])
            P.op("dve", "tensor_tensor", out=t64[:], in0=pso[0:64, :], in1=rl[:], op=ALU.mult,
                 reads=[rpso, rrl], writes=[rt64])
            P.op("dve", "tensor_tensor", out=og[:], in0=t64[:], in1=sg[:], op=ALU.mult, reads=[rt64, rsg],
                 writes=[rog])
            P.dma("sp", ogT[h * 64:(h + 1) * 64, t * TT:(t + 1) * TT], og[:], reads=[rog], is_output=True)
    P.emit()
    return nc


_CACHE = {}


def _prog(key, fn):
    if key not in _CACHE:
        _CACHE[key] = fn()
    return _CACHE[key]


def _run(nc, ins):
    res = run_bass_kernel_spmd(nc, ins, core_ids=list(range(8)))
    return res.results


def _colvec(v):
    return np.ascontiguousarray(np.asarray(v, np.float32).reshape(8, 128).T)


def _ret_consts(hd, S):
    gamma = 1.0 - 2.0 ** (-5.0 - hd)
    lg = np.log(gamma)
    idx = np.arange(128)
    m = idx[:, None]
    n = idx[None, :]
    same = (m // 64) == (n // 64)
    mask = np.where(same, np.exp(lg * np.abs(n - m)) * np.exp(-lg * (n % 64 + 1)) / 16.0, 0.0)
    cv = np.zeros((128, 8), np.float32)
    cv[:, 0] = np.exp(lg * (63 - idx % 64)) / 16.0
    qd = np.exp(lg * (idx % 64 + 1))
    cv[:, 1] = qd
    cv[:, 2] = qd * qd
    cv[:, 3] = np.exp(lg * 64)
    cv[:, 4] = -0.5
    return mask.astype(np.float32), cv


def _rope_tables(S):
    inv = 1.0 / (10000.0 ** (np.arange(0, 256, 2) / 256.0))
    ang = np.arange(S)[None, :] * inv[:, None]
    return np.cos(ang).astype(np.float32), np.sin(ang).astype(np.float32)


def kernel(x, c, ada_w, ada_b, norm_g, ret_w_in, ret_w_o, kv_ada_w, kv_ada_b, kv_norm_g,
           fox_w_kv, fox_w_f, fox_b_f, fox_w_qg, fox_w_o, ffn_w_gate, ffn_w_up, ffn_w_down,
           router_w, router_b, moe_w_gate, moe_w_up, moe_w_down, final_ada_w, final_ada_b,
           final_norm_g):
    f32 = np.float32
    A = lambda a: np.asarray(a, f32)
    x, c = A(x), A(c)
    B, S, Dm = x.shape
    T = B * S
    ident_f = np.eye(128, dtype=f32)
    ident_b = ident_f.astype(NPBF)
    W_all = np.concatenate([A(ada_w[l]) for l in range(4)] + [A(kv_ada_w), A(final_ada_w)], axis=1)
    b_all = np.concatenate([A(ada_b[l]) for l in range(4)] + [A(kv_ada_b), A(final_ada_b)])
    cT = np.ascontiguousarray(c.T.reshape(8, 128, 2).transpose(1, 0, 2).reshape(128, 16))
    ins = []
    for i in range(8):
        ins.append({"cT": cT, "W": np.ascontiguousarray(W_all[:, i * 3584:(i + 1) * 3584]),
                    "bT": np.ascontiguousarray(b_all[i * 3584:(i + 1) * 3584].reshape(28, 128).T)})
    r = _run(_prog("mod", build_mod), ins)
    mods = np.concatenate([q["mod"].reshape(128, 28, 2).transpose(1, 0, 2).reshape(3584, 2) for q in r], 0)
    del W_all

    def modl(l, j, b):
        return mods[l * 6144 + j * 1024:l * 6144 + (j + 1) * 1024, b]
    kv_sh = lambda b: mods[24576:25600, b]
    kv_sc = lambda b: mods[25600:26624, b]
    f_sh = lambda b: mods[26624:27648, b]
    f_sc = lambda b: mods[27648:28672, b]
    norm_g = A(norm_g)
    zeros = np.zeros(1024, f32)

    xT = np.ascontiguousarray(x.reshape(T, Dm).T)
    ins = []
    for i in range(8):
        b = i // 4
        vec = np.concatenate([_colvec(norm_g[0, 0]), _colvec(modl(0, 1, b)), _colvec(modl(0, 0, b))], 1)
        ins.append({"hT": np.ascontiguousarray(xT[:, i * TC:(i + 1) * TC]), "vec": np.ascontiguousarray(vec)})
    r = _run(_prog("norm0", build_norm0), ins)
    aT = np.concatenate([q["aT"] for q in r], 1)
    hT = xT

    cosT, sinT = _rope_tables(S)
    idx = np.arange(128)
    tri = (idx[:, None] <= idx[None, :]).astype(f32)
    trimask = (idx[None, :] >= idx[:, None]).astype(f32).astype(NPBF)
    shs = np.zeros((128, 64), f32)
    for m_ in range(64):
        shs[m_ + 64, m_] = 1
    sel = np.zeros((8, 8, 128), f32)
    for e in range(8):
        sel[e, e, :] = 1
    sel = np.ascontiguousarray(sel.reshape(8, 1024))

    def post(l, mixT, KC, moe, final, extra_kv, wo, wg, wu, wd, rw=None, rb=None):
        FF = wg.shape[-1]
        NFC = 4 if moe else 2
        nc = _prog(("post", KC, moe, final, extra_kv), lambda: build_post(KC, moe, final, extra_kv, FF, NFC))
        ins = []
        for i in range(8):
            b = i // 4
            if final:
                ngN, scN, shN = A(final_norm_g), f_sc(b), f_sh(b)
            else:
                ngN, scN, shN = norm_g[l + 1, 0], modl(l + 1, 1, b), modl(l + 1, 0, b)
            if extra_kv:
                ngK, scK, shK = A(kv_norm_g), kv_sc(b), kv_sh(b)
            else:
                ngK, scK, shK = zeros, zeros, zeros
            groups = [modl(l, 2, b), norm_g[l, 1], modl(l, 4, b), modl(l, 3, b), modl(l, 5, b),
                      ngN, scN, shN, ngK, scK, shK]
            vec = np.ascontiguousarray(np.concatenate([_colvec(g) for g in groups], 1))
            d = {"hT": np.ascontiguousarray(hT[:, i * TC:(i + 1) * TC]),
                 "mixT": np.ascontiguousarray(mixT[:, i * TC:(i + 1) * TC]),
                 "wo": wo, "wg": wg, "wu": wu, "wd": wd, "vec": vec}
            if moe:
                d.update({"rw": rw, "rb": np.ascontiguousarray(np.tile(A(rb)[None, :], (128, 1))), "sel": sel,
                          "ident": ident_f})
            ins.append(d)
        return _run(nc, ins)

    for l in range(2):
        w_in = A(ret_w_in[l])
        ins = []
        for i in range(8):
            b, hd = i // 4, i % 4
            win = np.concatenate([w_in[:, hd * 256:(hd + 1) * 256], w_in[:, 1024 + hd * 256:1024 + (hd + 1) * 256],
                                  w_in[:, 2048 + hd * 512:2048 + (hd + 1) * 512],
                                  w_in[:, 4096 + hd * 512:4096 + (hd + 1) * 512]], 1)
            mask, cv = _ret_consts(hd, S)
            ins.append({"aT": np.ascontiguousarray(aT[:, b * S:(b + 1) * S]), "win": np.ascontiguousarray(win),
                        "cosT": cosT, "sinT": sinT, "maskbd": mask, "cvec": cv, "identb": ident_b})
        r = _run(_prog("ret", build_ret), ins)
        mixT = np.empty((2048, T), NPBF)
        for i in range(8):
            b, hd = i // 4, i % 4
            mixT[hd * 512:(hd + 1) * 512, b * S:(b + 1) * S] = r[i]["ygT"]
        if l == 0:
            r = post(0, mixT, 16, False, False, False, A(ret_w_o[0]), A(ffn_w_gate[0])[None], A(ffn_w_up[0])[None],
                     A(ffn_w_down[0])[None])
        else:
            r = post(1, mixT, 16, True, False, True, A(ret_w_o[1]), A(moe_w_gate[0]), A(moe_w_up[0]),
                     A(moe_w_down[0]), A(router_w[0]), router_b[0])
            kvT = np.concatenate([q["kvT"] for q in r], 1)
        hT = np.concatenate([q["houtT"] for q in r], 1)
        aT = np.concatenate([q["anT"] for q in r], 1)

    w_kv, w_f, b_f = A(fox_w_kv), A(fox_w_f), A(fox_b_f)
    out = None
    for j in range(2):
        l = 2 + j
        w_qg = A(fox_w_qg[j])
        ins = []
        for i in range(8):
            b, hg = i // 4, i % 4
            wq = np.zeros((1024, 268), f32)
            wvf = np.zeros((1024, 260), f32)
            for h in range(4):
                gh = hg * 4 + h
                wq[:, h * 67:h * 67 + 64] = w_qg[:, gh * 64:(gh + 1) * 64]
                wvf[:, h * 65:h * 65 + 64] = w_kv[:, 1024 + gh * 64:1024 + (gh + 1) * 64]
                wvf[:, h * 65 + 64] = w_f[:, gh]
            ins.append({"aT": np.ascontiguousarray(aT[:, b * S:(b + 1) * S]),
                        "kvT": np.ascontiguousarray(kvT[:, b * S:(b + 1) * S]),
                        "wq": wq, "wgt": np.ascontiguousarray(w_qg[:, 1024 + hg * 256:1024 + (hg + 1) * 256]),
                        "wk": np.ascontiguousarray(w_kv[:, hg * 256:(hg + 1) * 256]), "wvf": wvf,
                        "bf": np.ascontiguousarray(np.tile(b_f[None, hg * 4:(hg + 1) * 4], (128, 1))),
                        "tri": tri, "trimask": trimask, "identb": ident_b, "shiftsel": shs})
        r = _run(_prog("attn", build_attn), ins)
        mixT = np.empty((1024, T), NPBF)
        for i in range(8):
            b, hg = i // 4, i % 4
            mixT[hg * 256:(hg + 1) * 256, b * S:(b + 1) * S] = r[i]["ogT"]
        if j == 0:
            r = post(2, mixT, 8, False, False, False, A(fox_w_o[0]), A(ffn_w_gate[1])[None], A(ffn_w_up[1])[None],
                     A(ffn_w_down[1])[None])
            hT = np.concatenate([q["houtT"] for q in r], 1)
            aT = np.concatenate([q["anT"] for q in r], 1)
        else:
            r = post(3, mixT, 8, True, True, False, A(fox_w_o[1]), A(moe_w_gate[1]), A(moe_w_up[1]),
                     A(moe_w_down[1]), A(router_w[1]), router_b[1])
            outT = np.concatenate([q["outT"] for q in r], 1)
            out = np.ascontiguousarray(outT.T).reshape(B, S, Dm).astype(f32)
    return out
```

```python
import contextlib
import numpy as np
import ml_dtypes
import concourse.bass as bass
import concourse.mybir as mybir
from concourse.bass_utils import run_bass_kernel_spmd

F32 = mybir.dt.float32
BF16 = mybir.dt.bfloat16
ALU = mybir.AluOpType
AF = mybir.ActivationFunctionType
AX = mybir.AxisListType
NPBF = ml_dtypes.bfloat16

ENGS = ["pe", "act", "dve", "pool", "sp"]
SEG = 6000
SELF_SYNC = True


class Res:
    _n = 0

    def __init__(self, name="r"):
        Res._n += 1
        self.name = f"{name}_{Res._n}"
        self.last_w = None
        self.readers = []


class Prog:
    def __init__(self, nc):
        self.nc = nc
        self.ops = {e: [] for e in ENGS}
        self.seq = {e: 0 for e in ENGS}
        self.known = {e: {} for e in ENGS}
        self.semkeys = {}
        self.dma_cnt = {}
        self.stack = contextlib.ExitStack()
        self.out_waits = []
        self._nt = 0

    def sbuf(self, shape, dtype, name=None):
        self._nt += 1
        return self.stack.enter_context(
            self.nc.sbuf_tensor(name or f"sb{self._nt}", list(shape), dtype))

    def psum(self, shape, dtype=F32, name=None):
        self._nt += 1
        return self.stack.enter_context(
            self.nc.psum_tensor(name or f"ps{self._nt}", list(shape), dtype))

    def _deps(self, eng, reads, writes):
        deps = []
        for r in reads:
            if r.last_w is not None:
                deps.append(r.last_w)
        for w in writes:
            if w.last_w is not None:
                deps.append(w.last_w)
            deps.extend(w.readers)
        waits = {}
        for key, val, deng in deps:
            if deng == eng and (eng in ("pe",) or not SELF_SYNC):
                continue
            if self.known[eng].get(key, 0) >= val:
                continue
            if waits.get(key, 0) < val:
                waits[key] = val
        for k, v in waits.items():
            self.known[eng][k] = v
            self.semkeys[k] = None
        return list(waits.items())

    def op(self, eng, meth, *args, reads=(), writes=(), **kw):
        waits = self._deps(eng, reads, writes)
        s = self.seq[eng]
        self.seq[eng] += 1
        key = ("eng", eng, s // SEG)
        val = s % SEG + 1
        self.semkeys[key] = None
        tok = (key, val, eng)
        for r in reads:
            r.readers.append(tok)
        for w in writes:
            w.last_w = tok
            w.readers = []
        self.ops[eng].append((waits, meth, args, kw, (key, 1)))
        return tok

    def dma(self, q, out, in_, reads=(), writes=(), is_output=False, **kw):
        waits = self._deps(q, reads, writes)
        anchor = writes[0] if writes else reads[0]
        key = ("dma", anchor.name, "w" if writes else "r")
        self.dma_cnt[key] = self.dma_cnt.get(key, 0) + 16
        val = self.dma_cnt[key]
        self.semkeys[key] = None
        tok = (key, val, "dma")
        for r in reads:
            r.readers.append(tok)
        for w in writes:
            w.last_w = tok
            w.readers = []
        kw = dict(kw)
        kw["out"] = out
        kw["in_"] = in_
        self.ops[q].append((waits, "dma_start", (), kw, (key, 16)))
        if is_output:
            self.out_waits.append((key, val))
        return tok

    def mm(self, out, lhsT, rhs, start, stop, reads, writes):
        return self.op("pe", "matmul", out, lhsT, rhs, start=start, stop=stop,
                       reads=reads, writes=writes)

    def emit(self):
        nc = self.nc
        sems = {}
        for i, k in enumerate(self.semkeys):
            sems[k] = self.stack.enter_context(nc.semaphore(f"s{i}"))
        fin = {}
        for k, v in self.out_waits:
            fin[k] = max(fin.get(k, 0), v)
        ops = self.ops

        def run(engh, ename):
            for waits, meth, args, kw, (ikey, inc) in ops[ename]:
                for k, v in waits:
                    engh.wait_ge(sems[k], v)
                getattr(engh, meth)(*args, **kw).then_inc(sems[ikey], inc)
            if ename == "sp":
                for k, v in fin.items():
                    engh.wait_ge(sems[k], v)

        with nc.Block() as block:
            @block.sync
            def _(e):
                run(e, "sp")

            @block.scalar
            def _(e):
                run(e, "act")

            @block.vector
            def _(e):
                run(e, "dve")

            @block.gpsimd
            def _(e):
                run(e, "pool")

            @block.tensor
            def _(e):
                run(e, "pe")
        self.stack.close()
        print("ops:", {e: len(v) for e, v in ops.items()}, "sems:", len(sems), flush=True)
        return nc


class PsumPool:
    def __init__(self, P, n):
        self.banks = [(P.psum([128, 512], F32), Res("psb")) for _ in range(n)]
        self.i = 0

    def get(self):
        b = self.banks[self.i % len(self.banks)]
        self.i += 1
        return b


D = 1024
EPS = 1e-6
TC = 4096
TT = 512


def build_mod():
    nc = bass.Bass("TRN2", target_bir_lowering=False)
    cT = nc.dram_tensor("cT", [128, 16], F32, kind="ExternalInput").ap()
    W = nc.dram_tensor("W", [1024, 3584], F32, kind="ExternalInput").ap()
    bT = nc.dram_tensor("bT", [128, 28], F32, kind="ExternalInput").ap()
    out = nc.dram_tensor("mod", [128, 56], F32, kind="ExternalOutput").ap()
    P = Prog(nc)
    c_sb = P.sbuf([128, 16], F32); rc = Res()
    ca = P.sbuf([128, 16], F32); rca = Res()
    b_sb = P.sbuf([128, 28], F32); rb = Res()
    wt = P.sbuf([128, 8, 3584], F32); rw = [Res() for _ in range(8)]
    o_sb = P.sbuf([128, 28, 2], F32); ro = Res()
    ps = P.psum([128, 28, 2], F32); rps = Res()
    P.dma("sp", c_sb[:], cT[:, :], writes=[rc])
    P.dma("sp", b_sb[:], bT[:, :], writes=[rb])
    for k in range(8):
        P.dma("sp", wt[:, k, :], W[k * 128:(k + 1) * 128, :], writes=[rw[k]])
    P.op("act", "activation", out=ca[:], in_=c_sb[:], func=AF.Silu, reads=[rc], writes=[rca])
    for j in range(28):
        for k in range(8):
            P.mm(ps[:, j, :], wt[:, k, j * 128:(j + 1) * 128], ca[:, 2 * k:2 * k + 2],
                 start=(k == 0), stop=(k == 7), reads=[rw[k], rca], writes=[rps])
    for b in range(2):
        P.op("dve", "tensor_tensor", out=o_sb[:, :, b], in0=ps[:, :, b], in1=b_sb[:, :], op=ALU.add,
             reads=[rps, rb], writes=[ro])
    P.dma("sp", out[:, :], o_sb[:].rearrange("p a b -> p (a b)"), reads=[ro], is_output=True)
    P.emit()
    return nc


class Ctx:
    pass


def setup_common(P, c):
    c.ones_bf = P.sbuf([128, 128], BF16)
    c.r_ones = Res()
    P.op("dve", "memset", c.ones_bf[:], 1.0, writes=[c.r_ones])
    c.eps_t = P.sbuf([128, 1], F32)
    c.r_eps = Res()
    P.op("dve", "memset", c.eps_t[:], EPS, writes=[c.r_eps])


def make_gs(P, c, vec, rvec, col_ng, col_sc):
    gs = P.sbuf([128, 8], F32)
    rg = Res()
    P.op("dve", "scalar_tensor_tensor", out=gs[:], in0=vec[:, col_sc:col_sc + 8], scalar=1.0,
         in1=vec[:, col_ng:col_ng + 8], op0=ALU.add, op1=ALU.mult, reads=[rvec], writes=[rg])
    return gs, rg


def emit_norm(P, c, pp, h, rh, toks, gs, rgs, sh_ap, rsh, out, rout, scr, otoks=None):
    if otoks is None:
        otoks = toks
    sq, rsq = scr["sq"]
    P.op("act", "activation", out=sq[:], in_=h[:, :, toks], func=AF.Square, reads=list(rh), writes=[rsq])
    ps, rps = pp.get()
    for k in range(8):
        P.mm(ps[:], c.ones_bf[:], sq[:, k, :], start=(k == 0), stop=(k == 7),
             reads=[c.r_ones, rsq], writes=[rps])
    ln, rln = scr["ln"]
    P.op("act", "activation", out=ln[:], in_=ps[:], func=AF.Ln, scale=1.0 / D, bias=c.eps_t[:],
         reads=[rps, c.r_eps], writes=[rln])
    rstd, rrstd = scr["rstd"]
    P.op("act", "activation", out=rstd[:], in_=ln[:], func=AF.Exp, scale=-0.5, reads=[rln], writes=[rrstd])
    for k in range(8):
        u, ru = scr["u"][k % 2]
        P.op("dve", "tensor_tensor", out=u[:], in0=h[:, k, toks], in1=rstd[:], op=ALU.mult,
             reads=[rh[k], rrstd], writes=[ru])
        P.op("act", "activation", out=out[:, k, otoks], in_=u[:], func=AF.Identity,
             scale=gs[:, k:k + 1], bias=sh_ap(k), reads=[ru, rgs, rsh], writes=[rout[k]])


def make_scr(P):
    return {
        "sq": (P.sbuf([128, 8, TT], BF16), Res()),
        "ln": (P.sbuf([128, TT], F32), Res()),
        "rstd": (P.sbuf([128, TT], F32), Res()),
        "u": [(P.sbuf([128, TT], F32), Res()) for _ in range(2)],
    }


def build_norm0():
    nc = bass.Bass("TRN2", target_bir_lowering=False)
    hT = nc.dram_tensor("hT", [D, TC], F32, kind="ExternalInput").ap()
    vecd = nc.dram_tensor("vec", [128, 24], F32, kind="ExternalInput").ap()
    aT = nc.dram_tensor("aT", [D, TC], BF16, kind="ExternalOutput").ap()
    hTv = hT.rearrange("(k p) t -> p k t", p=128)
    aTv = aT.rearrange("(k p) t -> p k t", p=128)
    P = Prog(nc)
    c = Ctx()
    setup_common(P, c)
    pp = PsumPool(P, 4)
    vec = P.sbuf([128, 24], F32); rvec = Res()
    P.dma("sp", vec[:], vecd[:, :], writes=[rvec])
    gs, rgs = make_gs(P, c, vec, rvec, 0, 8)
    scr = make_scr(P)
    hb = [(P.sbuf([128, 8, TT], F32), [Res() for _ in range(8)]) for _ in range(2)]
    ab = [(P.sbuf([128, 8, TT], BF16), [Res() for _ in range(8)]) for _ in range(2)]
    for t in range(TC // TT):
        h, rh = hb[t % 2]
        a, ra = ab[t % 2]
        P.dma("sp", h[:], hTv[:, :, t * TT:(t + 1) * TT], writes=rh)
        emit_norm(P, c, pp, h, rh, slice(0, TT), gs, rgs, lambda k: vec[:, 16 + k:17 + k], rvec, a, ra, scr)
        P.dma("sp", aTv[:, :, t * TT:(t + 1) * TT], a[:], reads=ra, is_output=True)
    P.emit()
    return nc


def build_post(KC, moe, final, extra_kv, FF, NFC):
    E = 8 if moe else 1
    NP = 4
    PT = TC // NP
    NTT = PT // TT
    NFG = FF // (NFC * 128)
    GW = NFC * 128
    nc = bass.Bass("TRN2", target_bir_lowering=False)
    hT = nc.dram_tensor("hT", [D, TC], F32, kind="ExternalInput").ap()
    mixT = nc.dram_tensor("mixT", [KC * 128, TC], BF16, kind="ExternalInput").ap()
    wo = nc.dram_tensor("wo", [KC * 128, D], F32, kind="ExternalInput").ap()
    wg = nc.dram_tensor("wg", [E, D, FF], F32, kind="ExternalInput").ap()
    wu = nc.dram_tensor("wu", [E, D, FF], F32, kind="ExternalInput").ap()
    wd = nc.dram_tensor("wd", [E, FF, D], F32, kind="ExternalInput").ap()
    vecd = nc.dram_tensor("vec", [128, 88], F32, kind="ExternalInput").ap()
    if moe:
        rwd = nc.dram_tensor("rw", [D, 8], F32, kind="ExternalInput").ap()
        rbd = nc.dram_tensor("rb", [128, 8], F32, kind="ExternalInput").ap()
        seld = nc.dram_tensor("sel", [8, 8 * 128], F32, kind="ExternalInput").ap()
        identd = nc.dram_tensor("ident", [128, 128], F32, kind="ExternalInput").ap()
    if final:
        outT = nc.dram_tensor("outT", [D, TC], F32, kind="ExternalOutput").ap()
    else:
        houtT = nc.dram_tensor("houtT", [D, TC], F32, kind="ExternalOutput").ap()
        anT = nc.dram_tensor("anT", [D, TC], BF16, kind="ExternalOutput").ap()
    if extra_kv:
        kvT = nc.dram_tensor("kvT", [D, TC], BF16, kind="ExternalOutput").ap()
    fm = lambda ap: ap.rearrange("(k p) t -> p k t", p=128)
    hTv, mixTv = fm(hT), fm(mixT)

    P = Prog(nc)
    c = Ctx()
    setup_common(P, c)
    pp = PsumPool(P, 7)
    vec = P.sbuf([128, 88], F32); rvec = Res()
    P.dma("sp", vec[:], vecd[:, :], writes=[rvec])
    col = lambda g, k: vec[:, g * 8 + k:g * 8 + k + 1]
    gs2, rgs2 = make_gs(P, c, vec, rvec, 8, 16)
    gsN, rgsN = make_gs(P, c, vec, rvec, 40, 48)
    if extra_kv:
        gsK, rgsK = make_gs(P, c, vec, rvec, 64, 72)
    scr = make_scr(P)

    wo_sb = P.sbuf([128, KC, D], BF16); rwo = Res()
    P.dma("pool", wo_sb[:], wo.rearrange("(k p) d -> p k d", p=128), writes=[rwo])

    if moe:
        rw_sb = P.sbuf([128, 8, 8], BF16); rrw = Res()
        P.dma("pool", rw_sb[:], rwd.rearrange("(k p) e -> p k e", p=128), writes=[rrw])
        rb_sb = P.sbuf([128, 8], F32); rrb = Res()
        P.dma("sp", rb_sb[:], rbd[:, :], writes=[rrb])
        sel_sb = P.sbuf([8, 8 * 128], F32); rsel = Res()
        P.dma("sp", sel_sb[:], seld[:, :], writes=[rsel])
        ident = P.sbuf([128, 128], F32); rid = Res()
        P.dma("sp", ident[:], identd[:, :], writes=[rid])
        gT = P.sbuf([8, PT], F32); rgT = Res()
        gb = [(P.sbuf([128, PT], BF16), Res()) for _ in range(2)]
        small = P.psum([128, 512], F32); rsmall = Res()
        _st = {}

        def stile(name, shape):
            if name not in _st:
                _st[name] = (P.sbuf(shape, F32), Res())
            return _st[name]

    hbuf = P.sbuf([128, 8, PT], F32)
    rh = [[Res() for _ in range(8)] for _ in range(NTT)]
    mbuf = P.sbuf([128, 8, PT], BF16)
    rm = [[Res() for _ in range(8)] for _ in range(NTT)]
    mixb = (P.sbuf([128, KC, TT], BF16), Res())
    wgb = [(P.sbuf([128, 8, GW], BF16), Res()) for _ in range(2)]
    wub = [(P.sbuf([128, 8, GW], BF16), Res()) for _ in range(2)]
    wdb = [(P.sbuf([128, NFC, D], BF16), Res()) for _ in range(2)]
    actb = [(P.sbuf([128, NFC, TT], BF16), [Res() for _ in range(NFC)]) for _ in range(2)]
    sil = [(P.sbuf([128, TT], BF16), Res()) for _ in range(3)]
    tmpb = [(P.sbuf([128, TT], BF16), Res()) for _ in range(2)]
    outb = [(P.sbuf([128, 8, TT], F32 if final else BF16), [Res() for _ in range(8)]) for _ in range(2)]
    nsil = [0]
    nob = [0]

    for ps_i in range(NP):
        t0 = ps_i * PT
        for tt in range(NTT):
            toks = slice(tt * TT, (tt + 1) * TT)
            g0 = t0 + tt * TT
            P.dma("sp", hbuf[:, :, toks], hTv[:, :, g0:g0 + TT], writes=rh[tt])
            mx, rmx = mixb
            P.dma("sp", mx[:], mixTv[:, :, g0:g0 + TT], writes=[rmx])
            for d in range(8):
                ps, rps = pp.get()
                for k in range(KC):
                    P.mm(ps[:], wo_sb[:, k, d * 128:(d + 1) * 128], mx[:, k, :], start=(k == 0),
                         stop=(k == KC - 1), reads=[rwo, rmx], writes=[rps])
                P.op("dve", "scalar_tensor_tensor", out=hbuf[:, d, toks], in0=ps[:], scalar=col(0, d),
                     in1=hbuf[:, d, toks], op0=ALU.mult, op1=ALU.add,
                     reads=[rps, rvec, rh[tt][d]], writes=[rh[tt][d]])
            emit_norm(P, c, pp, hbuf, rh[tt], toks, gs2, rgs2, lambda k: col(3, k), rvec, mbuf, rm[tt], scr)
            if moe:
                for sb in range(TT // 128):
                    tk = slice(tt * TT + sb * 128, tt * TT + (sb + 1) * 128)
                    for k in range(8):
                        P.mm(small[:, 0:8], mbuf[:, k, tk], rw_sb[:, k, :], start=(k == 0), stop=(k == 7),
                             reads=[rm[tt][k], rrw], writes=[rsmall])
                    lg, rlg = stile("lg", [128, 8])
                    P.op("dve", "tensor_tensor", out=lg[:], in0=small[:, 0:8], in1=rb_sb[:], op=ALU.add,
                         reads=[rsmall, rrb], writes=[rlg])
                    m1, rm1 = stile("m1", [128, 1])
                    P.op("dve", "reduce_max", out=m1[:], in_=lg[:], axis=AX.X, reads=[rlg], writes=[rm1])
                    k1, rk1 = stile("k1", [128, 8])
                    P.op("dve", "tensor_scalar", out=k1[:], in0=lg[:], scalar1=m1[:, 0:1], scalar2=None,
                         op0=ALU.is_equal, reads=[rlg, rm1], writes=[rk1])
                    lg2, rlg2 = stile("lg2", [128, 8])
                    P.op("dve", "scalar_tensor_tensor", out=lg2[:], in0=k1[:], scalar=-1e30, in1=lg[:],
                         op0=ALU.mult, op1=ALU.add, reads=[rk1, rlg], writes=[rlg2])
                    m2, rm2 = stile("m2", [128, 1])
                    P.op("dve", "reduce_max", out=m2[:], in_=lg2[:], axis=AX.X, reads=[rlg2], writes=[rm2])
                    k2, rk2 = stile("k2", [128, 8])
                    P.op("dve", "tensor_scalar", out=k2[:], in0=lg2[:], scalar1=m2[:, 0:1], scalar2=None,
                         op0=ALU.is_equal, reads=[rlg2, rm2], writes=[rk2])
                    dd, rdd = stile("dd", [128, 1])
                    P.op("dve", "tensor_tensor", out=dd[:], in0=m2[:], in1=m1[:], op=ALU.subtract,
                         reads=[rm1, rm2], writes=[rdd])
                    ee, ree = stile("ee", [128, 1])
                    P.op("act", "activation", out=ee[:], in_=dd[:], func=AF.Exp, reads=[rdd], writes=[ree])
                    den, rden = stile("den", [128, 1])
                    P.op("dve", "tensor_scalar", out=den[:], in0=ee[:], scalar1=1.0, scalar2=None, op0=ALU.add,
                         reads=[ree], writes=[rden])
                    w1, rw1 = stile("w1", [128, 1])
                    P.op("dve", "reciprocal", out=w1[:], in_=den[:], reads=[rden], writes=[rw1])
                    w2, rw2 = stile("w2", [128, 1])
                    P.op("dve", "tensor_tensor", out=w2[:], in0=ee[:], in1=w1[:], op=ALU.mult,
                         reads=[ree, rw1], writes=[rw2])
                    t1, rt1 = stile("t1", [128, 8])
                    P.op("dve", "tensor_scalar", out=t1[:], in0=k1[:], scalar1=w1[:, 0:1], scalar2=None,
                         op0=ALU.mult, reads=[rk1, rw1], writes=[rt1])
                    gt, rgt = stile("gt", [128, 8])
                    P.op("dve", "scalar_tensor_tensor", out=gt[:], in0=k2[:], scalar=w2[:, 0:1], in1=t1[:],
                         op0=ALU.mult, op1=ALU.add, reads=[rk2, rw2, rt1], writes=[rgt])
                    P.op("pe", "transpose", small[0:8, 128:256], gt[:], ident[:], reads=[rgt, rid],
                         writes=[rsmall])
                    P.op("dve", "tensor_copy", out=gT[:, tk], in_=small[0:8, 128:256], reads=[rsmall],
                         writes=[rgT])
        steps = [(e, fg, tt) for e in range(E) for fg in range(NFG) for tt in range(NTT)]
        pend = None
        wslot = [0]

        def load_w(e, fg):
            i = wslot[0] % 2
            wslot[0] += 1
            g_, rg_ = wgb[i]; u_, ru_ = wub[i]; d_, rd_ = wdb[i]
            P.dma("pool", g_[:], wg[e, :, fg * GW:(fg + 1) * GW].rearrange("(k p) f -> p k f", p=128),
                  writes=[rg_])
            P.dma("pool", u_[:], wu[e, :, fg * GW:(fg + 1) * GW].rearrange("(k p) f -> p k f", p=128),
                  writes=[ru_])
            P.dma("pool", d_[:], wd[e, fg * GW:(fg + 1) * GW, :].rearrange("(c p) d -> p c d", p=128),
                  writes=[rd_])
            return i

        def down(e, fg, tt, wi, ai):
            toks = slice(tt * TT, (tt + 1) * TT)
            d_, rd_ = wdb[wi]
            a_, ra_ = actb[ai]
            for d in range(8):
                ps, rps = pp.get()
                for fc in range(NFC):
                    P.mm(ps[:], d_[:, fc, d * 128:(d + 1) * 128], a_[:, fc, :], start=(fc == 0),
                         stop=(fc == NFC - 1), reads=[rd_, ra_[fc]], writes=[rps])
                P.op("dve", "scalar_tensor_tensor", out=hbuf[:, d, toks], in0=ps[:], scalar=col(4, d),
                     in1=hbuf[:, d, toks], op0=ALU.mult, op1=ALU.add,
                     reads=[rps, rvec, rh[tt][d]], writes=[rh[tt][d]])

        cur_w = None
        cur_key = None
        astep = 0
        cur_gb = None
        for (e, fg, tt) in steps:
            toks = slice(tt * TT, (tt + 1) * TT)
            if moe and fg == 0 and tt == 0:
                gbt, rgb = gb[e % 2]
                for t2 in range(NTT):
                    tk2 = slice(t2 * TT, (t2 + 1) * TT)
                    ps, rps = pp.get()
                    P.mm(ps[:], sel_sb[:, e * 128:(e + 1) * 128], gT[:, tk2], start=True, stop=True,
                         reads=[rsel, rgT], writes=[rps])
                    P.op("act", "activation", out=gbt[:, tk2], in_=ps[:], func=AF.Identity,
                         reads=[rps], writes=[rgb])
                cur_gb = (gbt, rgb)
            if cur_key != (e, fg):
                cur_w = load_w(e, fg)
                cur_key = (e, fg)
            g_, rg_ = wgb[cur_w]; u_, ru_ = wub[cur_w]
            ai = astep % 2
            astep += 1
            a_, ra_ = actb[ai]
            for fc in range(NFC):
                psg, rpsg = pp.get()
                for k in range(8):
                    P.mm(psg[:], g_[:, k, fc * 128:(fc + 1) * 128], mbuf[:, k, toks], start=(k == 0),
                         stop=(k == 7), reads=[rg_, rm[tt][k]], writes=[rpsg])
                psu, rpsu = pp.get()
                for k in range(8):
                    P.mm(psu[:], u_[:, k, fc * 128:(fc + 1) * 128], mbuf[:, k, toks], start=(k == 0),
                         stop=(k == 7), reads=[ru_, rm[tt][k]], writes=[rpsu])
                s_, rs_ = sil[nsil[0] % 3]
                nsil[0] += 1
                P.op("act", "activation", out=s_[:], in_=psg[:], func=AF.Silu, reads=[rpsg], writes=[rs_])
                if moe:
                    t_, rt_ = tmpb[fc % 2]
                    P.op("dve", "tensor_tensor", out=t_[:], in0=psu[:], in1=s_[:], op=ALU.mult,
                         reads=[rpsu, rs_], writes=[rt_])
                    P.op("dve", "tensor_tensor", out=a_[:, fc, :], in0=t_[:], in1=cur_gb[0][:, toks],
                         op=ALU.mult, reads=[rt_, cur_gb[1]], writes=[ra_[fc]])
                else:
                    P.op("dve", "tensor_tensor", out=a_[:, fc, :], in0=psu[:], in1=s_[:], op=ALU.mult,
                         reads=[rpsu, rs_], writes=[ra_[fc]])
            if pend is not None:
                down(*pend)
            pend = (e, fg, tt, cur_w, ai)
        down(*pend)
        for tt in range(NTT):
            toks = slice(tt * TT, (tt + 1) * TT)
            g0 = t0 + tt * TT
            if not final:
                P.dma("sp", fm(houtT)[:, :, g0:g0 + TT], hbuf[:, :, toks], reads=rh[tt], is_output=True)
            o_, ro_ = outb[nob[0] % 2]; nob[0] += 1
            emit_norm(P, c, pp, hbuf, rh[tt], toks, gsN, rgsN, lambda k: col(7, k), rvec, o_, ro_, scr,
                      otoks=slice(0, TT))
            P.dma("sp", fm(outT if final else anT)[:, :, g0:g0 + TT], o_[:], reads=ro_, is_output=True)
            if extra_kv:
                o_, ro_ = outb[nob[0] % 2]; nob[0] += 1
                emit_norm(P, c, pp, hbuf, rh[tt], toks, gsK, rgsK, lambda k: col(10, k), rvec, o_, ro_, scr,
                          otoks=slice(0, TT))
                P.dma("sp", fm(kvT)[:, :, g0:g0 + TT], o_[:], reads=ro_, is_output=True)
    P.emit()
    return nc


S_LEN = 16384


def build_ret(NTILES=S_LEN // TT):
    nc = bass.Bass("TRN2", target_bir_lowering=False)
    S = NTILES * TT
    aT = nc.dram_tensor("aT", [D, S], BF16, kind="ExternalInput").ap()
    win = nc.dram_tensor("win", [D, 1536], F32, kind="ExternalInput").ap()
    cosd = nc.dram_tensor("cosT", [128, S], F32, kind="ExternalInput").ap()
    sind = nc.dram_tensor("sinT", [128, S], F32, kind="ExternalInput").ap()
    maskd = nc.dram_tensor("maskbd", [128, 128], F32, kind="ExternalInput").ap()
    cvd = nc.dram_tensor("cvec", [128, 8], F32, kind="ExternalInput").ap()
    identd = nc.dram_tensor("identb", [128, 128], BF16, kind="ExternalInput").ap()
    ygT = nc.dram_tensor("ygT", [512, S], BF16, kind="ExternalOutput").ap()
    aTv = aT.rearrange("(k p) t -> p k t", p=128)
    ygTv = ygT.rearrange("(j p) t -> p j t", p=128)

    P = Prog(nc)
    pp = PsumPool(P, 4)
    w_sb = P.sbuf([128, 8, 1536], BF16); rw = Res()
    P.dma("pool", w_sb[:], win.rearrange("(k p) f -> p k f", p=128), writes=[rw])
    mask = P.sbuf([128, 128], F32); rmask = Res()
    P.dma("sp", mask[:], maskd[:, :], writes=[rmask])
    cv = P.sbuf([128, 8], F32); rcv = Res()
    P.dma("sp", cv[:], cvd[:, :], writes=[rcv])
    ident = P.sbuf([128, 128], BF16); rid = Res()
    P.dma("sp", ident[:], identd[:, :], writes=[rid])
    KD, QD, QD2, CDEC, NH = [cv[:, i:i + 1] for i in range(5)]

    ab = [(P.sbuf([128, 8, TT], BF16), Res()) for _ in range(2)]
    cosb = [(P.sbuf([128, TT], F32), Res()) for _ in range(2)]
    sinb = [(P.sbuf([128, TT], F32), Res()) for _ in range(2)]
    qkf = [(P.sbuf([128, TT], F32), Res()) for _ in range(4)]
    rt = [(P.sbuf([128, TT], F32), Res()) for _ in range(4)]
    qTb = [(P.sbuf([128, 2, TT], BF16), [Res(), Res()]) for _ in range(2)]
    kTb = [(P.sbuf([128, 2, TT], BF16), [Res(), Res()]) for _ in range(2)]
    vb = [(P.sbuf([128, 4, 512], BF16), [Res() for _ in range(4)]) for _ in range(2)]
    gb = [(P.sbuf([128, 4, 512], BF16), [Res() for _ in range(4)]) for _ in range(2)]
    ktm = [(P.sbuf([128, 4, 256], BF16), [Res() for _ in range(4)]) for _ in range(2)]
    pTb = [(P.sbuf([128, 128], BF16), Res()) for _ in range(2)]
    st = P.sbuf([128, 2, 512], F32); rst = [Res(), Res()]
    stb = [(P.sbuf([128, 2, 512], BF16), Res()) for _ in range(2)]
    yb = [(P.sbuf([128, 512], BF16), Res()) for _ in range(2)]
    ygb = [(P.sbuf([128, 512], BF16), Res()) for _ in range(2)]
    ygTb = [(P.sbuf([128, 4, TT], BF16), [Res() for _ in range(4)]) for _ in range(2)]
    stats = P.sbuf([128, 6], F32); rstats = Res()
    mv = P.sbuf([128, 2], F32); rmv = Res()
    vv = P.sbuf([128, 1], F32); rvv = Res()
    rs = P.sbuf([128, 1], F32); rrs = Res()
    scl = P.sbuf([128, 1], F32); rscl = Res()
    ps_kv = [(P.psum([128, 512], F32), Res()) for _ in range(2)]
    ps_s = P.psum([128, 512], F32); rps_s = Res()
    ps_t = P.psum([128, 1024], BF16); rps_t = [Res(), Res()]

    P.op("dve", "memset", st[:], 0.0, writes=rst)
    P.op("dve", "memset", stb[1][0][:], 0.0, writes=[stb[1][1]])
    nstb = [1]

    for t in range(NTILES):
        a, ra = ab[t % 2]
        co, rco = cosb[t % 2]
        si, rsi = sinb[t % 2]
        P.dma("sp", a[:], aTv[:, :, t * TT:(t + 1) * TT], writes=[ra])
        P.dma("sp", co[:], cosd[:, t * TT:(t + 1) * TT], writes=[rco])
        P.dma("sp", si[:], sind[:, t * TT:(t + 1) * TT], writes=[rsi])
        for j in range(4):
            ps, rps = pp.get()
            for k in range(8):
                P.mm(ps[:], w_sb[:, k, j * 128:(j + 1) * 128], a[:, k, :], start=(k == 0), stop=(k == 7),
                     reads=[rw, ra], writes=[rps])
            f, rf = qkf[j]
            P.op("act", "activation", out=f[:], in_=ps[:], func=AF.Identity, reads=[rps], writes=[rf])
        qT, rqT = qTb[t % 2]
        kT, rkT = kTb[t % 2]
        for (eng, A, B, dst, rdst, tmp) in (("dve", qkf[0], qkf[1], qT, rqT, rt[0:2]),
                                            ("pool", qkf[2], qkf[3], kT, rkT, rt[2:4])):
            (fa, rfa), (fb, rfb) = A, B
            (t1, rt1), (t2, rt2) = tmp
            P.op(eng, "tensor_tensor", out=t1[:], in0=fa[:], in1=co[:], op=ALU.mult, reads=[rfa, rco], writes=[rt1])
            P.op(eng, "tensor_tensor", out=t2[:], in0=fb[:], in1=si[:], op=ALU.mult, reads=[rfb, rsi], writes=[rt2])
            P.op(eng, "tensor_tensor", out=dst[:, 0, :], in0=t1[:], in1=t2[:], op=ALU.subtract,
                 reads=[rt1, rt2], writes=[rdst[0]])
            P.op(eng, "tensor_tensor", out=t1[:], in0=fb[:], in1=co[:], op=ALU.mult, reads=[rfb, rco], writes=[rt1])
            P.op(eng, "tensor_tensor", out=t2[:], in0=fa[:], in1=si[:], op=ALU.mult, reads=[rfa, rsi], writes=[rt2])
            P.op(eng, "tensor_tensor", out=dst[:, 1, :], in0=t1[:], in1=t2[:], op=ALU.add,
                 reads=[rt1, rt2], writes=[rdst[1]])
        v, rv = vb[t % 2]
        g, rg = gb[t % 2]
        for blk in range(4):
            bs = slice(blk * 128, (blk + 1) * 128)
            ps, rps = pp.get()
            for k in range(8):
                P.mm(ps[:], a[:, k, bs], w_sb[:, k, 512:1024], start=(k == 0), stop=(k == 7),
                     reads=[rw, ra], writes=[rps])
            P.op("act", "activation", out=v[:, blk, :], in_=ps[:], func=AF.Identity, reads=[rps], writes=[rv[blk]])
            ps, rps = pp.get()
            for k in range(8):
                P.mm(ps[:], a[:, k, bs], w_sb[:, k, 1024:1536], start=(k == 0), stop=(k == 7),
                     reads=[rw, ra], writes=[rps])
            P.op("act", "activation", out=g[:, blk, :], in_=ps[:], func=AF.Silu, reads=[rps], writes=[rg[blk]])
        km, rkm = ktm[t % 2]
        for blk in range(4):
            bs = slice(blk * 128, (blk + 1) * 128)
            for kc in range(2):
                P.op("pe", "transpose", ps_t[:, kc * 128:(kc + 1) * 128], kT[:, kc, bs], ident[:],
                     reads=[rkT[kc], rid], writes=[rps_t[0]])
            P.op("act", "activation", out=km[:, blk, :], in_=ps_t[:, 0:256], func=AF.Identity, scale=KD,
                 reads=[rps_t[0], rcv], writes=[rkm[blk]])
        ygT_sb, rygT = ygTb[t % 2]
        for blk in range(4):
            bs = slice(blk * 128, (blk + 1) * 128)
            for kc in range(2):
                P.mm(ps_s[:, 0:128], kT[:, kc, bs], qT[:, kc, bs], start=(kc == 0), stop=(kc == 1),
                     reads=[rkT[kc], rqT[kc]], writes=[rps_s])
            pT, rpT = pTb[blk % 2]
            P.op("dve", "tensor_tensor", out=pT[:], in0=ps_s[:, 0:128], in1=mask[:], op=ALU.mult,
                 reads=[rps_s, rmask], writes=[rpT])
            pso, rpso = pp.get()
            P.mm(pso[:], pT[:], v[:, blk, :], start=True, stop=False, reads=[rpT, rv[blk]], writes=[rpso])
            for half in range(2):
                o = half * 64
                cs = slice(blk * 128 + o, blk * 128 + o + 64)
                sb_, rsb_ = stb[nstb[0]]
                for kc in range(2):
                    P.mm(pso[o:o + 64, :], qT[:, kc, cs], sb_[:, kc, :], start=False,
                         stop=(kc == 1 and half == 1), reads=[rqT[kc], rsb_], writes=[rpso])
                for kc in range(2):
                    pk, rpk = ps_kv[kc]
                    P.mm(pk[:], km[o:o + 64, blk, kc * 128:(kc + 1) * 128], v[o:o + 64, blk, :], start=True,
                         stop=True, reads=[rkm[blk], rv[blk]], writes=[rpk])
                    P.op("dve", "scalar_tensor_tensor", out=st[:, kc, :], in0=st[:, kc, :], scalar=CDEC,
                         in1=pk[:], op0=ALU.mult, op1=ALU.add, reads=[rst[kc], rcv, rpk], writes=[rst[kc]])
                nstb[0] ^= 1
                sb2, rsb2 = stb[nstb[0]]
                P.op("act", "activation", out=sb2[:], in_=st[:], func=AF.Identity, reads=rst, writes=[rsb2])
            P.op("dve", "bn_stats", out=stats[:], in_=pso[:], reads=[rpso], writes=[rstats])
            P.op("dve", "bn_aggr", out=mv[:], in_=stats[:], reads=[rstats], writes=[rmv])
            P.op("dve", "tensor_scalar", out=vv[:], in0=mv[:, 1:2], scalar1=QD2, scalar2=EPS, op0=ALU.mult,
                 op1=ALU.add, reads=[rmv, rcv], writes=[rvv])
            P.op("pool", "tensor_tensor", out=rs[:], in0=vv[:], in1=NH, op=ALU.pow, reads=[rvv, rcv],
                 writes=[rrs])
            P.op("pool", "tensor_tensor", out=scl[:], in0=rs[:], in1=QD, op=ALU.mult, reads=[rrs, rcv],
                 writes=[rscl])
            y, ry = yb[blk % 2]
            P.op("dve", "tensor_scalar", out=y[:], in0=pso[:], scalar1=mv[:, 0:1], scalar2=scl[:, 0:1],
                 op0=ALU.subtract, op1=ALU.mult, reads=[rpso, rmv, rscl], writes=[ry])
            yg, ryg = ygb[blk % 2]
            P.op("pool", "tensor_tensor", out=yg[:], in0=y[:], in1=g[:, blk, :], op=ALU.mult,
                 reads=[ry, rg[blk]], writes=[ryg])
            for j in range(4):
                P.op("pe", "transpose", ps_t[:, 512 + j * 128:512 + (j + 1) * 128], yg[:, j * 128:(j + 1) * 128],
                     ident[:], reads=[ryg, rid], writes=[rps_t[1]])
            P.op("act", "activation", out=ygT_sb[:, :, bs],
                 in_=ps_t[:, 512:1024].rearrange("p (j t) -> p j t", j=4), func=AF.Identity,
                 reads=[rps_t[1]], writes=[rygT[blk]])
        P.dma("sp", ygTv[:, :, t * TT:(t + 1) * TT], ygT_sb[:], reads=rygT, is_output=True)
    P.emit()
    return nc


def build_attn(NQT=S_LEN // TT, stage=9, NH=4):
    nc = bass.Bass("TRN2", target_bir_lowering=False)
    S = NQT * TT
    NB = S // 128
    aT = nc.dram_tensor("aT", [D, S], BF16, kind="ExternalInput").ap()
    kvT = nc.dram_tensor("kvT", [D, S], BF16, kind="ExternalInput").ap()
    wqd = nc.dram_tensor("wq", [D, 268], F32, kind="ExternalInput").ap()
    wgd = nc.dram_tensor("wgt", [D, 256], F32, kind="ExternalInput").ap()
    wkd = nc.dram_tensor("wk", [D, 256], F32, kind="ExternalInput").ap()
    wvfd = nc.dram_tensor("wvf", [D, 260], F32, kind="ExternalInput").ap()
    bfd = nc.dram_tensor("bf", [128, 4], F32, kind="ExternalInput").ap()
    trid = nc.dram_tensor("tri", [128, 128], F32, kind="ExternalInput").ap()
    tmd = nc.dram_tensor("trimask", [128, 128], BF16, kind="ExternalInput").ap()
    idbd = nc.dram_tensor("identb", [128, 128], BF16, kind="ExternalInput").ap()
    shd = nc.dram_tensor("shiftsel", [128, 64], F32, kind="ExternalInput").ap()
    ogT = nc.dram_tensor("ogT", [256, S], BF16, kind="ExternalOutput").ap()
    fm = lambda ap: ap.rearrange("(k p) t -> p k t", p=128)
    aTv, kvTv = fm(aT), fm(kvT)

    P = Prog(nc)
    ppS = PsumPool(P, 3)
    ppB = PsumPool(P, 3)
    ps_o = [(P.psum([128, 512], F32), Res()) for _ in range(2)]

    def wload(dram, n):
        t_ = P.sbuf([128, 8, n], BF16); r_ = Res()
        P.dma("pool", t_[:], dram.rearrange("(k p) f -> p k f", p=128), writes=[r_])
        return t_, r_
    wq, rwq = wload(wqd, 268)
    wg, rwg = wload(wgd, 256)
    wk, rwk = wload(wkd, 256)
    wvf, rwvf = wload(wvfd, 260)
    bft = P.sbuf([128, 4], F32); rbf = Res()
    P.dma("sp", bft[:], bfd[:, :], writes=[rbf])
    negb = P.sbuf([128, 4], F32); rnegb = Res()
    P.op("dve", "tensor_scalar", out=negb[:], in0=bft[:], scalar1=-1.0, scalar2=None, op0=ALU.mult,
         reads=[rbf], writes=[rnegb])
    tri = P.sbuf([128, 128], F32); rtri = Res()
    P.dma("sp", tri[:], trid[:, :], writes=[rtri])
    trimask = P.sbuf([128, 128], BF16); rtm = Res()
    P.dma("sp", trimask[:], tmd[:, :], writes=[rtm])
    identb = P.sbuf([128, 128], BF16); ridb = Res()
    P.dma("sp", identb[:], idbd[:, :], writes=[ridb])
    shiftsel = P.sbuf([128, 64], F32); rshs = Res()
    P.dma("sp", shiftsel[:], shd[:, :], writes=[rshs])
    onesf = P.sbuf([128, 128], F32); ronesf = Res()
    P.op("dve", "memset", onesf[:], 1.0, writes=[ronesf])

    K_aug = P.sbuf([67, S], BF16); rK = [Res() for _ in range(NQT)]
    rKones = Res()
    P.op("dve", "memset", K_aug[:], 8.0, writes=[rKones])
    V_aug = P.sbuf([128, NB, 128], BF16); rV = [Res() for _ in range(NB)]
    rVones = Res()
    P.op("dve", "memset", V_aug[:], 1.0, writes=[rVones])
    e_all = P.sbuf([128, NB], F32); re_all = Res()
    sp_all = P.sbuf([128, NB], F32); rsp = Res()
    tot = P.sbuf([128, NB], F32); rtot = Res()
    cs = P.sbuf([128, NB], F32); rcs = Res()
    tmpF = P.sbuf([128, NB], F32); rtmpF = Res()
    negF = P.sbuf([128, NB], F32); rnegF = Res()
    Pc = P.sbuf([128, NB, 67], BF16); rPc = Res()
    P.op("dve", "memset", Pc[:], 0.0, writes=[rPc])
    r1 = P.sbuf([128, NB], F32); rr1 = Res()
    r2 = P.sbuf([128, NB], F32); rr2 = Res()
    onesNB = P.sbuf([128, NB], F32); ronesNB = Res()
    P.op("dve", "memset", onesNB[:], 1.0, writes=[ronesNB])

    kvb = [(P.sbuf([128, 8, TT], BF16), Res()) for _ in range(2)]
    ab = [(P.sbuf([128, 8, TT], BF16), Res()) for _ in range(2)]
    Qb = [(P.sbuf([67, TT], BF16), Res()) for _ in range(2)]
    pTb = [(P.sbuf([128, TT], BF16), Res()) for _ in range(3)]
    eg = P.sbuf([64, TT], F32); reg = Res()
    sgb = [(P.sbuf([64, TT], F32), Res()) for _ in range(2)]
    lr = P.sbuf([128, TT], F32); rlr = Res()
    rl = P.sbuf([64, TT], F32); rrl = Res()
    t64 = P.sbuf([64, TT], F32); rt64 = Res()
    ogb = [(P.sbuf([64, TT], BF16), Res()) for _ in range(2)]
    nkv = [0]
    na = [0]
    npt = [0]

    for h in range(NH):
        hs = slice(h * 64, (h + 1) * 64)
        for tt in range(NQT):
            kv, rkv = kvb[nkv[0] % 2]; nkv[0] += 1
            P.dma("sp", kv[:], kvTv[:, :, tt * TT:(tt + 1) * TT], writes=[rkv])
            ps, rps = ppB.get()
            for k in range(8):
                P.mm(ps[0:64, :], wk[:, k, hs], kv[:, k, :], start=(k == 0), stop=(k == 7),
                     reads=[rwk, rkv], writes=[rps])
            P.op("act", "activation", out=K_aug[0:64, tt * TT:(tt + 1) * TT], in_=ps[0:64, :], func=AF.Identity,
                 reads=[rps, rKones], writes=[rK[tt]])
            for blk in range(4):
                gb_ = tt * 4 + blk
                bs = slice(blk * 128, (blk + 1) * 128)
                ps, rps = ppB.get()
                for k in range(8):
                    P.mm(ps[:, 0:65], kv[:, k, bs], wvf[:, k, h * 65:(h + 1) * 65], start=(k == 0), stop=(k == 7),
                         reads=[rwvf, rkv], writes=[rps])
                P.op("act", "activation", out=V_aug[:, gb_, 0:64], in_=ps[:, 0:64], func=AF.Identity,
                     reads=[rps, rVones], writes=[rV[gb_]])
                P.op("act", "activation", out=e_all[:, gb_:gb_ + 1], in_=ps[:, 64:65], func=AF.Exp, scale=-1.0,
                     bias=negb[:, h:h + 1], reads=[rps, rnegb], writes=[re_all])
        P.op("act", "activation", out=sp_all[:], in_=e_all[:], func=AF.Ln, bias=onesf[:, 0:1],
             reads=[re_all, ronesf], writes=[rsp])
        psF, rpsF = ppB.get()
        P.mm(psF[:, 0:NB], tri[:], sp_all[:], start=True, stop=True, reads=[rtri, rsp], writes=[rpsF])
        psT, rpsT = ppB.get()
        P.mm(psT[:, 0:NB], onesf[:], sp_all[:], start=True, stop=True, reads=[ronesf, rsp], writes=[rpsT])
        P.op("dve", "tensor_copy", out=tot[:], in_=psT[:, 0:NB], reads=[rpsT], writes=[rtot])
        P.op("dve", "tensor_tensor_scan", out=cs[:], data0=onesNB[:], data1=tot[:], initial=0.0,
             op0=ALU.mult, op1=ALU.add, reads=[ronesNB, rtot], writes=[rcs])
        P.op("dve", "tensor_tensor", out=tmpF[:], in0=cs[:], in1=tot[:], op=ALU.subtract,
             reads=[rcs, rtot], writes=[rtmpF])
        P.op("dve", "tensor_tensor", out=negF[:], in0=psF[:, 0:NB], in1=tmpF[:], op=ALU.add,
             reads=[rpsF, rtmpF], writes=[rnegF])
        P.op("dve", "tensor_scalar", out=Pc[:, :, 64], in0=negF[:], scalar1=-1.0, scalar2=None, op0=ALU.mult,
             reads=[rnegF], writes=[rPc])
        P.op("dve", "scalar_tensor_tensor", out=r1[:], in0=negF[:], scalar=-1.0, in1=Pc[:, :, 64], op0=ALU.mult,
             op1=ALU.subtract, reads=[rnegF, rPc], writes=[rr1])
        P.op("dve", "tensor_copy", out=Pc[:, :, 65], in_=r1[:], reads=[rr1], writes=[rPc])
        P.op("dve", "tensor_tensor", out=r2[:], in0=r1[:], in1=Pc[:, :, 65], op=ALU.subtract,
             reads=[rr1, rPc], writes=[rr2])
        P.op("dve", "tensor_copy", out=Pc[:, :, 66], in_=r2[:], reads=[rr2], writes=[rPc])

        if stage == 1:
            og, rog = ogb[0]
            P.op("dve", "tensor_copy", out=og[:, 0:NB], in_=negF[0:64, :], reads=[rnegF, rPc], writes=[rog])
            P.dma("sp", ogT[h * 64:(h + 1) * 64, 0:TT], og[:], reads=[rog], is_output=True)
            continue
        steps = [(t, j) for t in range(NQT) for j in range(4 * t + 4)]
        tstate = {}
        sstate = {}
        LOOK = 2

        def qprep(t):
            a, ra = ab[na[0] % 2]
            Q, rQ = Qb[na[0] % 2]
            sg, rsg = sgb[na[0] % 2]
            og, rog = ogb[na[0] % 2]
            pso, rpso = ps_o[na[0] % 2]
            na[0] += 1
            tstate[t] = (Q, rQ, sg, rsg, og, rog, pso, rpso)
            P.dma("sp", a[:], aTv[:, :, t * TT:(t + 1) * TT], writes=[ra])
            ps, rps = ppB.get()
            for k in range(8):
                P.mm(ps[0:67, :], wq[:, k, h * 67:(h + 1) * 67], a[:, k, :], start=(k == 0), stop=False,
                     reads=[rwq, ra], writes=[rps])
            for blk in range(4):
                P.mm(ps[0:67, blk * 128:(blk + 1) * 128], Pc[:, t * 4 + blk, :], identb[:], start=False,
                     stop=(blk == 3), reads=[rPc, ridb], writes=[rps])
            P.op("act", "activation", out=Q[:], in_=ps[0:67, :], func=AF.Identity, reads=[rps], writes=[rQ])
            ps2, rps2 = ppB.get()
            for k in range(8):
                P.mm(ps2[0:64, :], wg[:, k, hs], a[:, k, :], start=(k == 0), stop=(k == 7),
                     reads=[rwg, ra], writes=[rps2])
            P.op("act", "activation", out=eg[:], in_=ps2[0:64, :], func=AF.Exp, scale=-1.0, reads=[rps2],
                 writes=[reg])
            P.op("dve", "tensor_scalar", out=eg[:], in0=eg[:], scalar1=1.0, scalar2=None, op0=ALU.add,
                 reads=[reg], writes=[reg])
            P.op("dve", "reciprocal", out=sg[:], in_=eg[:], reads=[reg], writes=[rsg])

        def emit_qk(idx):
            t, j = steps[idx]
            Q, rQ = tstate[t][0:2]
            o = j - 4 * t
            c0 = 128 * o if o > 0 else 0
            N = TT - c0
            pss, rpss = ppS.get()
            P.mm(pss[:, 0:N], K_aug[0:67, j * 128:(j + 1) * 128], Q[0:67, c0:TT], start=True, stop=True,
                 reads=[rK[j // 4], rKones, rQ], writes=[rpss])
            pT, rpT = pTb[npt[0] % 3]; npt[0] += 1
            P.op("act", "activation", out=pT[:, 0:N], in_=pss[:, 0:N], func=AF.Exp, scale=0.125,
                 bias=negF[:, j:j + 1], reads=[rpss, rnegF], writes=[rpT])
            if o >= 0:
                P.op("pool", "tensor_tensor", out=pT[:, 0:128], in0=pT[:, 0:128], in1=trimask[:], op=ALU.mult,
                     reads=[rpT, rtm], writes=[rpT])
            sstate[idx] = (pT, rpT, c0, N)

        def emit_pv(idx):
            t, j = steps[idx]
            Q, rQ, sg, rsg, og, rog, pso, rpso = tstate[t]
            pT, rpT, c0, N = sstate.pop(idx)
            nblk = 4 * t + 4
            P.mm(pso[:, c0:TT], V_aug[:, j, :], pT[:, 0:N], start=(j == 0), stop=(j == nblk - 1),
                 reads=[rV[j], rVones, rpT], writes=[rpso])
            if j == 0 and t + 1 < NQT:
                qprep(t + 1)
            if j == nblk - 1:
                P.op("act", "activation", out=lr[:], in_=pso[:], func=AF.Identity, reads=[rpso],
                     writes=[rlr])
                psb, rpsb = ppB.get()
                P.mm(psb[0:64, :], shiftsel[:], lr[:], start=True, stop=True, reads=[rshs, rlr],
                     writes=[rpsb])
                P.op("dve", "reciprocal", out=rl[:], in_=psb[0:64, :], reads=[rpsb], writes=[rrl])
                P.op("dve", "tensor_tensor", out=t64[:], in0=pso[0:64, :], in1=rl[:], op=ALU.mult,
                     reads=[rpso, rrl], writes=[rt64])
                P.op("dve", "tensor_tensor", out=og[:], in0=t64[:], in1=sg[:], op=ALU.mult, reads=[rt64, rsg],
                     writes=[rog])
                P.dma("sp", ogT[h * 64:(h + 1) * 64, t * TT:(t + 1) * TT], og[:], reads=[rog], is_output=True)
                del tstate[t]

        qprep(0)
        for idx in range(len(steps) + LOOK):
            if idx < len(steps):
                emit_qk(idx)
            if idx - LOOK >= 0:
                emit_pv(idx - LOOK)
    P.emit()
    return nc


_CACHE = {}


def _prog(key, fn):
    if key not in _CACHE:
        _CACHE[key] = fn()
    return _CACHE[key]


def _run(nc, ins):
    res = run_bass_kernel_spmd(nc, ins, core_ids=list(range(8)))
    return res.results


def _colvec(v):
    return np.ascontiguousarray(np.asarray(v, np.float32).reshape(8, 128).T)


def _ret_consts(hd, S):
    gamma = 1.0 - 2.0 ** (-5.0 - hd)
    lg = np.log(gamma)
    idx = np.arange(128)
    m = idx[:, None]
    n = idx[None, :]
    same = (m // 64) == (n // 64)
    mask = np.where(same, np.exp(lg * np.abs(n - m)) * np.exp(-lg * (n % 64 + 1)) / 16.0, 0.0)
    cv = np.zeros((128, 8), np.float32)
    cv[:, 0] = np.exp(lg * (63 - idx % 64)) / 16.0
    qd = np.exp(lg * (idx % 64 + 1))
    cv[:, 1] = qd
    cv[:, 2] = qd * qd
    cv[:, 3] = np.exp(lg * 64)
    cv[:, 4] = -0.5
    return mask.astype(np.float32), cv


def _rope_tables(S):
    inv = 1.0 / (10000.0 ** (np.arange(0, 256, 2) / 256.0))
    ang = np.arange(S)[None, :] * inv[:, None]
    return np.cos(ang).astype(np.float32), np.sin(ang).astype(np.float32)


def kernel(x, c, ada_w, ada_b, norm_g, ret_w_in, ret_w_o, kv_ada_w, kv_ada_b, kv_norm_g,
           fox_w_kv, fox_w_f, fox_b_f, fox_w_qg, fox_w_o, ffn_w_gate, ffn_w_up, ffn_w_down,
           router_w, router_b, moe_w_gate, moe_w_up, moe_w_down, final_ada_w, final_ada_b,
           final_norm_g):
    f32 = np.float32
    A = lambda a: np.asarray(a, f32)
    x, c = A(x), A(c)
    B, S, Dm = x.shape
    T = B * S
    ident_f = np.eye(128, dtype=f32)
    ident_b = ident_f.astype(NPBF)
    W_all = np.concatenate([A(ada_w[l]) for l in range(4)] + [A(kv_ada_w), A(final_ada_w)], axis=1)
    b_all = np.concatenate([A(ada_b[l]) for l in range(4)] + [A(kv_ada_b), A(final_ada_b)])
    cT = np.ascontiguousarray(c.T.reshape(8, 128, 2).transpose(1, 0, 2).reshape(128, 16))
    ins = []
    for i in range(8):
        ins.append({"cT": cT, "W": np.ascontiguousarray(W_all[:, i * 3584:(i + 1) * 3584]),
                    "bT": np.ascontiguousarray(b_all[i * 3584:(i + 1) * 3584].reshape(28, 128).T)})
    r = _run(_prog("mod", build_mod), ins)
    mods = np.concatenate([q["mod"].reshape(128, 28, 2).transpose(1, 0, 2).reshape(3584, 2) for q in r], 0)
    del W_all

    def modl(l, j, b):
        return mods[l * 6144 + j * 1024:l * 6144 + (j + 1) * 1024, b]
    kv_sh = lambda b: mods[24576:25600, b]
    kv_sc = lambda b: mods[25600:26624, b]
    f_sh = lambda b: mods[26624:27648, b]
    f_sc = lambda b: mods[27648:28672, b]
    norm_g = A(norm_g)
    zeros = np.zeros(1024, f32)

    xT = np.ascontiguousarray(x.reshape(T, Dm).T)
    ins = []
    for i in range(8):
        b = i // 4
        vec = np.concatenate([_colvec(norm_g[0, 0]), _colvec(modl(0, 1, b)), _colvec(modl(0, 0, b))], 1)
        ins.append({"hT": np.ascontiguousarray(xT[:, i * TC:(i + 1) * TC]), "vec": np.ascontiguousarray(vec)})
    r = _run(_prog("norm0", build_norm0), ins)
    aT = np.concatenate([q["aT"] for q in r], 1)
    hT = xT

    cosT, sinT = _rope_tables(S)
    idx = np.arange(128)
    tri = (idx[:, None] <= idx[None, :]).astype(f32)
    trimask = (idx[None, :] >= idx[:, None]).astype(f32).astype(NPBF)
    shs = np.zeros((128, 64), f32)
    for m_ in range(64):
        shs[m_ + 64, m_] = 1
    sel = np.zeros((8, 8, 128), f32)
    for e in range(8):
        sel[e, e, :] = 1
    sel = np.ascontiguousarray(sel.reshape(8, 1024))

    def post(l, mixT, KC, moe, final, extra_kv, wo, wg, wu, wd, rw=None, rb=None):
        FF = wg.shape[-1]
        NFC = 4 if moe else 2
        nc = _prog(("post", KC, moe, final, extra_kv), lambda: build_post(KC, moe, final, extra_kv, FF, NFC))
        ins = []
        for i in range(8):
            b = i // 4
            if final:
                ngN, scN, shN = A(final_norm_g), f_sc(b), f_sh(b)
            else:
                ngN, scN, shN = norm_g[l + 1, 0], modl(l + 1, 1, b), modl(l + 1, 0, b)
            if extra_kv:
                ngK, scK, shK = A(kv_norm_g), kv_sc(b), kv_sh(b)
            else:
                ngK, scK, shK = zeros, zeros, zeros
            groups = [modl(l, 2, b), norm_g[l, 1], modl(l, 4, b), modl(l, 3, b), modl(l, 5, b),
                      ngN, scN, shN, ngK, scK, shK]
            vec = np.ascontiguousarray(np.concatenate([_colvec(g) for g in groups], 1))
            d = {"hT": np.ascontiguousarray(hT[:, i * TC:(i + 1) * TC]),
                 "mixT": np.ascontiguousarray(mixT[:, i * TC:(i + 1) * TC]),
                 "wo": wo, "wg": wg, "wu": wu, "wd": wd, "vec": vec}
            if moe:
                d.update({"rw": rw, "rb": np.ascontiguousarray(np.tile(A(rb)[None, :], (128, 1))), "sel": sel,
                          "ident": ident_f})
            ins.append(d)
        return _run(nc, ins)

    for l in range(2):
        w_in = A(ret_w_in[l])
        ins = []
        for i in range(8):
            b, hd = i // 4, i % 4
            win = np.concatenate([w_in[:, hd * 256:(hd + 1) * 256], w_in[:, 1024 + hd * 256:1024 + (hd + 1) * 256],
                                  w_in[:, 2048 + hd * 512:2048 + (hd + 1) * 512],
                                  w_in[:, 4096 + hd * 512:4096 + (hd + 1) * 512]], 1)
            mask, cv = _ret_consts(hd, S)
            ins.append({"aT": np.ascontiguousarray(aT[:, b * S:(b + 1) * S]), "win": np.ascontiguousarray(win),
                        "cosT": cosT, "sinT": sinT, "maskbd": mask, "cvec": cv, "identb": ident_b})
        r = _run(_prog("ret", build_ret), ins)
        mixT = np.empty((2048, T), NPBF)
        for i in range(8):
            b, hd = i // 4, i % 4
            mixT[hd * 512:(hd + 1) * 512, b * S:(b + 1) * S] = r[i]["ygT"]
        if l == 0:
            r = post(0, mixT, 16, False, False, False, A(ret_w_o[0]), A(ffn_w_gate[0])[None], A(ffn_w_up[0])[None],
                     A(ffn_w_down[0])[None])
        else:
            r = post(1, mixT, 16, True, False, True, A(ret_w_o[1]), A(moe_w_gate[0]), A(moe_w_up[0]),
                     A(moe_w_down[0]), A(router_w[0]), router_b[0])
            kvT = np.concatenate([q["kvT"] for q in r], 1)
        hT = np.concatenate([q["houtT"] for q in r], 1)
        aT = np.concatenate([q["anT"] for q in r], 1)

    w_kv, w_f, b_f = A(fox_w_kv), A(fox_w_f), A(fox_b_f)
    out = None
    for j in range(2):
        l = 2 + j
        w_qg = A(fox_w_qg[j])
        ins = []
        for i in range(8):
            b, hg = i // 4, i % 4
            wq = np.zeros((1024, 268), f32)
            wvf = np.zeros((1024, 260), f32)
            for h in range(4):
                gh = hg * 4 + h
                wq[:, h * 67:h * 67 + 64] = w_qg[:, gh * 64:(gh + 1) * 64]
                wvf[:, h * 65:h * 65 + 64] = w_kv[:, 1024 + gh * 64:1024 + (gh + 1) * 64]
                wvf[:, h * 65 + 64] = w_f[:, gh]
            ins.append({"aT": np.ascontiguousarray(aT[:, b * S:(b + 1) * S]),
                        "kvT": np.ascontiguousarray(kvT[:, b * S:(b + 1) * S]),
                        "wq": wq, "wgt": np.ascontiguousarray(w_qg[:, 1024 + hg * 256:1024 + (hg + 1) * 256]),
                        "wk": np.ascontiguousarray(w_kv[:, hg * 256:(hg + 1) * 256]), "wvf": wvf,
                        "bf": np.ascontiguousarray(np.tile(b_f[None, hg * 4:(hg + 1) * 4], (128, 1))),
                        "tri": tri, "trimask": trimask, "identb": ident_b, "shiftsel": shs})
        r = _run(_prog("attn", build_attn), ins)
        mixT = np.empty((1024, T), NPBF)
        for i in range(8):
            b, hg = i // 4, i % 4
            mixT[hg * 256:(hg + 1) * 256, b * S:(b + 1) * S] = r[i]["ogT"]
        if j == 0:
            r = post(2, mixT, 8, False, False, False, A(fox_w_o[0]), A(ffn_w_gate[1])[None], A(ffn_w_up[1])[None],
                     A(ffn_w_down[1])[None])
            hT = np.concatenate([q["houtT"] for q in r], 1)
            aT = np.concatenate([q["anT"] for q in r], 1)
        else:
            r = post(3, mixT, 8, True, True, False, A(fox_w_o[1]), A(moe_w_gate[1]), A(moe_w_up[1]),
                     A(moe_w_down[1]), A(router_w[1]), router_b[1])
            outT = np.concatenate([q["outT"] for q in r], 1)
            out = np.ascontiguousarray(outT.T).reshape(B, S, Dm).astype(f32)
    return out
```

```python
import contextlib
import numpy as np
import ml_dtypes
import concourse.bass as bass
import concourse.mybir as mybir
from concourse.bass_utils import run_bass_kernel_spmd

F32 = mybir.dt.float32
BF16 = mybir.dt.bfloat16
ALU = mybir.AluOpType
AF = mybir.ActivationFunctionType
AX = mybir.AxisListType
NPBF = ml_dtypes.bfloat16

ENGS = ["pe", "act", "dve", "pool", "sp"]
SEG = 6000
SELF_SYNC = True


class Res:
    _n = 0

    def __init__(self, name="r"):
        Res._n += 1
        self.name = f"{name}_{Res._n}"
        self.last_w = None
        self.readers = []


class Prog:
    def __init__(self, nc):
        self.nc = nc
        self.ops = {e: [] for e in ENGS}
        self.seq = {e: 0 for e in ENGS}
        self.known = {e: {} for e in ENGS}
        self.semkeys = {}
        self.dma_cnt = {}
        self.stack = contextlib.ExitStack()
        self.out_waits = []
        self._nt = 0

    def sbuf(self, shape, dtype, name=None):
        self._nt += 1
        return self.stack.enter_context(
            self.nc.sbuf_tensor(name or f"sb{self._nt}", list(shape), dtype))

    def psum(self, shape, dtype=F32, name=None):
        self._nt += 1
        return self.stack.enter_context(
            self.nc.psum_tensor(name or f"ps{self._nt}", list(shape), dtype))

    def _deps(self, eng, reads, writes):
        deps = []
        for r in reads:
            if r.last_w is not None:
                deps.append(r.last_w)
        for w in writes:
            if w.last_w is not None:
                deps.append(w.last_w)
            deps.extend(w.readers)
        waits = {}
        for key, val, deng in deps:
            if deng == eng and (eng in ("pe",) or not SELF_SYNC):
                continue
            if self.known[eng].get(key, 0) >= val:
                continue
            if waits.get(key, 0) < val:
                waits[key] = val
        for k, v in waits.items():
            self.known[eng][k] = v
            self.semkeys[k] = None
        return list(waits.items())

    def op(self, eng, meth, *args, reads=(), writes=(), **kw):
        waits = self._deps(eng, reads, writes)
        s = self.seq[eng]
        self.seq[eng] += 1
        key = ("eng", eng, s // SEG)
        val = s % SEG + 1
        self.semkeys[key] = None
        tok = (key, val, eng)
        for r in reads:
            r.readers.append(tok)
        for w in writes:
            w.last_w = tok
            w.readers = []
        self.ops[eng].append((waits, meth, args, kw, (key, 1)))
        return tok

    def dma(self, q, out, in_, reads=(), writes=(), is_output=False, **kw):
        waits = self._deps(q, reads, writes)
        anchor = writes[0] if writes else reads[0]
        key = ("dma", anchor.name, "w" if writes else "r")
        self.dma_cnt[key] = self.dma_cnt.get(key, 0) + 16
        val = self.dma_cnt[key]
        self.semkeys[key] = None
        tok = (key, val, "dma")
        for r in reads:
            r.readers.append(tok)
        for w in writes:
            w.last_w = tok
            w.readers = []
        kw = dict(kw)
        kw["out"] = out
        kw["in_"] = in_
        self.ops[q].append((waits, "dma_start", (), kw, (key, 16)))
        if is_output:
            self.out_waits.append((key, val))
        return tok

    def mm(self, out, lhsT, rhs, start, stop, reads, writes):
        return self.op("pe", "matmul", out, lhsT, rhs, start=start, stop=stop,
                       reads=reads, writes=writes)

    def emit(self):
        nc = self.nc
        sems = {}
        for i, k in enumerate(self.semkeys):
            sems[k] = self.stack.enter_context(nc.semaphore(f"s{i}"))
        fin = {}
        for k, v in self.out_waits:
            fin[k] = max(fin.get(k, 0), v)
        ops = self.ops

        def run(engh, ename):
            for waits, meth, args, kw, (ikey, inc) in ops[ename]:
                for k, v in waits:
                    engh.wait_ge(sems[k], v)
                getattr(engh, meth)(*args, **kw).then_inc(sems[ikey], inc)
            if ename == "sp":
                for k, v in fin.items():
                    engh.wait_ge(sems[k], v)

        with nc.Block() as block:
            @block.sync
            def _(e):
                run(e, "sp")

            @block.scalar
            def _(e):
                run(e, "act")

            @block.vector
            def _(e):
                run(e, "dve")

            @block.gpsimd
            def _(e):
                run(e, "pool")

            @block.tensor
            def _(e):
                run(e, "pe")
        self.stack.close()
        print("ops:", {e: len(v) for e, v in ops.items()}, "sems:", len(sems), flush=True)
        return nc


class PsumPool:
    def __init__(self, P, n):
        self.banks = [(P.psum([128, 512], F32), Res("psb")) for _ in range(n)]
        self.i = 0

    def get(self):
        b = self.banks[self.i % len(self.banks)]
        self.i += 1
        return b


D = 1024
EPS = 1e-6
TC = 4096
TT = 512


def build_mod():
    nc = bass.Bass("TRN2", target_bir_lowering=False)
    cT = nc.dram_tensor("cT", [128, 16], F32, kind="ExternalInput").ap()
    W = nc.dram_tensor("W", [1024, 3584], F32, kind="ExternalInput").ap()
    bT = nc.dram_tensor("bT", [128, 28], F32, kind="ExternalInput").ap()
    out = nc.dram_tensor("mod", [128, 56], F32, kind="ExternalOutput").ap()
    P = Prog(nc)
    c_sb = P.sbuf([128, 16], F32); rc = Res()
    ca = P.sbuf([128, 16], F32); rca = Res()
    b_sb = P.sbuf([128, 28], F32); rb = Res()
    wt = P.sbuf([128, 8, 3584], F32); rw = [Res() for _ in range(8)]
    o_sb = P.sbuf([128, 28, 2], F32); ro = Res()
    ps = P.psum([128, 28, 2], F32); rps = Res()
    P.dma("sp", c_sb[:], cT[:, :], writes=[rc])
    P.dma("sp", b_sb[:], bT[:, :], writes=[rb])
    for k in range(8):
        P.dma("sp", wt[:, k, :], W[k * 128:(k + 1) * 128, :], writes=[rw[k]])
    P.op("act", "activation", out=ca[:], in_=c_sb[:], func=AF.Silu, reads=[rc], writes=[rca])
    for j in range(28):
        for k in range(8):
            P.mm(ps[:, j, :], wt[:, k, j * 128:(j + 1) * 128], ca[:, 2 * k:2 * k + 2],
                 start=(k == 0), stop=(k == 7), reads=[rw[k], rca], writes=[rps])
    for b in range(2):
        P.op("dve", "tensor_tensor", out=o_sb[:, :, b], in0=ps[:, :, b], in1=b_sb[:, :], op=ALU.add,
             reads=[rps, rb], writes=[ro])
    P.dma("sp", out[:, :], o_sb[:].rearrange("p a b -> p (a b)"), reads=[ro], is_output=True)
    P.emit()
    return nc


class Ctx:
    pass


def setup_common(P, c):
    c.ones_bf = P.sbuf([128, 128], BF16)
    c.r_ones = Res()
    P.op("dve", "memset", c.ones_bf[:], 1.0, writes=[c.r_ones])
    c.eps_t = P.sbuf([128, 1], F32)
    c.r_eps = Res()
    P.op("dve", "memset", c.eps_t[:], EPS, writes=[c.r_eps])


def make_gs(P, c, vec, rvec, col_ng, col_sc):
    gs = P.sbuf([128, 8], F32)
    rg = Res()
    P.op("dve", "scalar_tensor_tensor", out=gs[:], in0=vec[:, col_sc:col_sc + 8], scalar=1.0,
         in1=vec[:, col_ng:col_ng + 8], op0=ALU.add, op1=ALU.mult, reads=[rvec], writes=[rg])
    return gs, rg


def emit_norm(P, c, pp, h, rh, toks, gs, rgs, sh_ap, rsh, out, rout, scr, otoks=None):
    if otoks is None:
        otoks = toks
    sq, rsq = scr["sq"]
    P.op("act", "activation", out=sq[:], in_=h[:, :, toks], func=AF.Square, reads=list(rh), writes=[rsq])
    ps, rps = pp.get()
    for k in range(8):
        P.mm(ps[:], c.ones_bf[:], sq[:, k, :], start=(k == 0), stop=(k == 7),
             reads=[c.r_ones, rsq], writes=[rps])
    ln, rln = scr["ln"]
    P.op("act", "activation", out=ln[:], in_=ps[:], func=AF.Ln, scale=1.0 / D, bias=c.eps_t[:],
         reads=[rps, c.r_eps], writes=[rln])
    rstd, rrstd = scr["rstd"]
    P.op("act", "activation", out=rstd[:], in_=ln[:], func=AF.Exp, scale=-0.5, reads=[rln], writes=[rrstd])
    for k in range(8):
        u, ru = scr["u"][k % 2]
        P.op("dve", "tensor_tensor", out=u[:], in0=h[:, k, toks], in1=rstd[:], op=ALU.mult,
             reads=[rh[k], rrstd], writes=[ru])
        P.op("act", "activation", out=out[:, k, otoks], in_=u[:], func=AF.Identity,
             scale=gs[:, k:k + 1], bias=sh_ap(k), reads=[ru, rgs, rsh], writes=[rout[k]])


def make_scr(P):
    return {
        "sq": (P.sbuf([128, 8, TT], BF16), Res()),
        "ln": (P.sbuf([128, TT], F32), Res()),
        "rstd": (P.sbuf([128, TT], F32), Res()),
        "u": [(P.sbuf([128, TT], F32), Res()) for _ in range(2)],
    }


def build_norm0():
    nc = bass.Bass("TRN2", target_bir_lowering=False)
    hT = nc.dram_tensor("hT", [D, TC], F32, kind="ExternalInput").ap()
    vecd = nc.dram_tensor("vec", [128, 24], F32, kind="ExternalInput").ap()
    aT = nc.dram_tensor("aT", [D, TC], BF16, kind="ExternalOutput").ap()
    hTv = hT.rearrange("(k p) t -> p k t", p=128)
    aTv = aT.rearrange("(k p) t -> p k t", p=128)
    P = Prog(nc)
    c = Ctx()
    setup_common(P, c)
    pp = PsumPool(P, 4)
    vec = P.sbuf([128, 24], F32); rvec = Res()
    P.dma("sp", vec[:], vecd[:, :], writes=[rvec])
    gs, rgs = make_gs(P, c, vec, rvec, 0, 8)
    scr = make_scr(P)
    hb = [(P.sbuf([128, 8, TT], F32), [Res() for _ in range(8)]) for _ in range(2)]
    ab = [(P.sbuf([128, 8, TT], BF16), [Res() for _ in range(8)]) for _ in range(2)]
    for t in range(TC // TT):
        h, rh = hb[t % 2]
        a, ra = ab[t % 2]
        P.dma("sp", h[:], hTv[:, :, t * TT:(t + 1) * TT], writes=rh)
        emit_norm(P, c, pp, h, rh, slice(0, TT), gs, rgs, lambda k: vec[:, 16 + k:17 + k], rvec, a, ra, scr)
        P.dma("sp", aTv[:, :, t * TT:(t + 1) * TT], a[:], reads=ra, is_output=True)
    P.emit()
    return nc


def build_post(KC, moe, final, extra_kv, FF, NFC):
    E = 8 if moe else 1
    NP = 4
    PT = TC // NP
    NTT = PT // TT
    NFG = FF // (NFC * 128)
    GW = NFC * 128
    nc = bass.Bass("TRN2", target_bir_lowering=False)
    hT = nc.dram_tensor("hT", [D, TC], F32, kind="ExternalInput").ap()
    mixT = nc.dram_tensor("mixT", [KC * 128, TC], BF16, kind="ExternalInput").ap()
    wo = nc.dram_tensor("wo", [KC * 128, D], F32, kind="ExternalInput").ap()
    wg = nc.dram_tensor("wg", [E, D, FF], F32, kind="ExternalInput").ap()
    wu = nc.dram_tensor("wu", [E, D, FF], F32, kind="ExternalInput").ap()
    wd = nc.dram_tensor("wd", [E, FF, D], F32, kind="ExternalInput").ap()
    vecd = nc.dram_tensor("vec", [128, 88], F32, kind="ExternalInput").ap()
    if moe:
        rwd = nc.dram_tensor("rw", [D, 8], F32, kind="ExternalInput").ap()
        rbd = nc.dram_tensor("rb", [128, 8], F32, kind="ExternalInput").ap()
        seld = nc.dram_tensor("sel", [8, 8 * 128], F32, kind="ExternalInput").ap()
        identd = nc.dram_tensor("ident", [128, 128], F32, kind="ExternalInput").ap()
    if final:
        outT = nc.dram_tensor("outT", [D, TC], F32, kind="ExternalOutput").ap()
    else:
        houtT = nc.dram_tensor("houtT", [D, TC], F32, kind="ExternalOutput").ap()
        anT = nc.dram_tensor("anT", [D, TC], BF16, kind="ExternalOutput").ap()
    if extra_kv:
        kvT = nc.dram_tensor("kvT", [D, TC], BF16, kind="ExternalOutput").ap()
    fm = lambda ap: ap.rearrange("(k p) t -> p k t", p=128)
    hTv, mixTv = fm(hT), fm(mixT)

    P = Prog(nc)
    c = Ctx()
    setup_common(P, c)
    pp = PsumPool(P, 7)
    vec = P.sbuf([128, 88], F32); rvec = Res()
    P.dma("sp", vec[:], vecd[:, :], writes=[rvec])
    col = lambda g, k: vec[:, g * 8 + k:g * 8 + k + 1]
    gs2, rgs2 = make_gs(P, c, vec, rvec, 8, 16)
    gsN, rgsN = make_gs(P, c, vec, rvec, 40, 48)
    if extra_kv:
        gsK, rgsK = make_gs(P, c, vec, rvec, 64, 72)
    scr = make_scr(P)

    wo_sb = P.sbuf([128, KC, D], BF16); rwo = Res()
    P.dma("pool", wo_sb[:], wo.rearrange("(k p) d -> p k d", p=128), writes=[rwo])

    if moe:
        rw_sb = P.sbuf([128, 8, 8], BF16); rrw = Res()
        P.dma("pool", rw_sb[:], rwd.rearrange("(k p) e -> p k e", p=128), writes=[rrw])
        rb_sb = P.sbuf([128, 8], F32); rrb = Res()
        P.dma("sp", rb_sb[:], rbd[:, :], writes=[rrb])
        sel_sb = P.sbuf([8, 8 * 128], F32); rsel = Res()
        P.dma("sp", sel_sb[:], seld[:, :], writes=[rsel])
        ident = P.sbuf([128, 128], F32); rid = Res()
        P.dma("sp", ident[:], identd[:, :], writes=[rid])
        gT = P.sbuf([8, PT], F32); rgT = Res()
        gb = [(P.sbuf([128, PT], BF16), Res()) for _ in range(2)]
        small = P.psum([128, 512], F32); rsmall = Res()
        _st = {}

        def stile(name, shape):
            if name not in _st:
                _st[name] = (P.sbuf(shape, F32), Res())
            return _st[name]

    hbuf = P.sbuf([128, 8, PT], F32)
    rh = [[Res() for _ in range(8)] for _ in range(NTT)]
    mbuf = P.sbuf([128, 8, PT], BF16)
    rm = [[Res() for _ in range(8)] for _ in range(NTT)]
    mixb = (P.sbuf([128, KC, TT], BF16), Res())
    wgb = [(P.sbuf([128, 8, GW], BF16), Res()) for _ in range(2)]
    wub = [(P.sbuf([128, 8, GW], BF16), Res()) for _ in range(2)]
    wdb = [(P.sbuf([128, NFC, D], BF16), Res()) for _ in range(2)]
    actb = [(P.sbuf([128, NFC, TT], BF16), [Res() for _ in range(NFC)]) for _ in range(2)]
    sil = [(P.sbuf([128, TT], BF16), Res()) for _ in range(3)]
    tmpb = [(P.sbuf([128, TT], BF16), Res()) for _ in range(2)]
    outb = [(P.sbuf([128, 8, TT], F32 if final else BF16), [Res() for _ in range(8)]) for _ in range(2)]
    nsil = [0]
    nob = [0]

    for ps_i in range(NP):
        t0 = ps_i * PT
        for tt in range(NTT):
            toks = slice(tt * TT, (tt + 1) * TT)
            g0 = t0 + tt * TT
            P.dma("sp", hbuf[:, :, toks], hTv[:, :, g0:g0 + TT], writes=rh[tt])
            mx, rmx = mixb
            P.dma("sp", mx[:], mixTv[:, :, g0:g0 + TT], writes=[rmx])
            for d in range(8):
                ps, rps = pp.get()
                for k in range(KC):
                    P.mm(ps[:], wo_sb[:, k, d * 128:(d + 1) * 128], mx[:, k, :], start=(k == 0),
                         stop=(k == KC - 1), reads=[rwo, rmx], writes=[rps])
                P.op("dve", "scalar_tensor_tensor", out=hbuf[:, d, toks], in0=ps[:], scalar=col(0, d),
                     in1=hbuf[:, d, toks], op0=ALU.mult, op1=ALU.add,
                     reads=[rps, rvec, rh[tt][d]], writes=[rh[tt][d]])
            emit_norm(P, c, pp, hbuf, rh[tt], toks, gs2, rgs2, lambda k: col(3, k), rvec, mbuf, rm[tt], scr)
            if moe:
                for sb in range(TT // 128):
                    tk = slice(tt * TT + sb * 128, tt * TT + (sb + 1) * 128)
                    for k in range(8):
                        P.mm(small[:, 0:8], mbuf[:, k, tk], rw_sb[:, k, :], start=(k == 0), stop=(k == 7),
                             reads=[rm[tt][k], rrw], writes=[rsmall])
                    lg, rlg = stile("lg", [128, 8])
                    P.op("dve", "tensor_tensor", out=lg[:], in0=small[:, 0:8], in1=rb_sb[:], op=ALU.add,
                         reads=[rsmall, rrb], writes=[rlg])
                    m1, rm1 = stile("m1", [128, 1])
                    P.op("dve", "reduce_max", out=m1[:], in_=lg[:], axis=AX.X, reads=[rlg], writes=[rm1])
                    k1, rk1 = stile("k1", [128, 8])
                    P.op("dve", "tensor_scalar", out=k1[:], in0=lg[:], scalar1=m1[:, 0:1], scalar2=None,
                         op0=ALU.is_equal, reads=[rlg, rm1], writes=[rk1])
                    lg2, rlg2 = stile("lg2", [128, 8])
                    P.op("dve", "scalar_tensor_tensor", out=lg2[:], in0=k1[:], scalar=-1e30, in1=lg[:],
                         op0=ALU.mult, op1=ALU.add, reads=[rk1, rlg], writes=[rlg2])
                    m2, rm2 = stile("m2", [128, 1])
                    P.op("dve", "reduce_max", out=m2[:], in_=lg2[:], axis=AX.X, reads=[rlg2], writes=[rm2])
                    k2, rk2 = stile("k2", [128, 8])
                    P.op("dve", "tensor_scalar", out=k2[:], in0=lg2[:], scalar1=m2[:, 0:1], scalar2=None,
                         op0=ALU.is_equal, reads=[rlg2, rm2], writes=[rk2])
                    dd, rdd = stile("dd", [128, 1])
                    P.op("dve", "tensor_tensor", out=dd[:], in0=m2[:], in1=m1[:], op=ALU.subtract,
                         reads=[rm1, rm2], writes=[rdd])
                    ee, ree = stile("ee", [128, 1])
                    P.op("act", "activation", out=ee[:], in_=dd[:], func=AF.Exp, reads=[rdd], writes=[ree])
                    den, rden = stile("den", [128, 1])
                    P.op("dve", "tensor_scalar", out=den[:], in0=ee[:], scalar1=1.0, scalar2=None, op0=ALU.add,
                         reads=[ree], writes=[rden])
                    w1, rw1 = stile("w1", [128, 1])
                    P.op("dve", "reciprocal", out=w1[:], in_=den[:], reads=[rden], writes=[rw1])
                    w2, rw2 = stile("w2", [128, 1])
                    P.op("dve", "tensor_tensor", out=w2[:], in0=ee[:], in1=w1[:], op=ALU.mult,
                         reads=[ree, rw1], writes=[rw2])
                    t1, rt1 = stile("t1", [128, 8])
                    P.op("dve", "tensor_scalar", out=t1[:], in0=k1[:], scalar1=w1[:, 0:1], scalar2=None,
                         op0=ALU.mult, reads=[rk1, rw1], writes=[rt1])
                    gt, rgt = stile("gt", [128, 8])
                    P.op("dve", "scalar_tensor_tensor", out=gt[:], in0=k2[:], scalar=w2[:, 0:1], in1=t1[:],
                         op0=ALU.mult, op1=ALU.add, reads=[rk2, rw2, rt1], writes=[rgt])
                    P.op("pe", "transpose", small[0:8, 128:256], gt[:], ident[:], reads=[rgt, rid],
                         writes=[rsmall])
                    P.op("dve", "tensor_copy", out=gT[:, tk], in_=small[0:8, 128:256], reads=[rsmall],
                         writes=[rgT])
        steps = [(e, fg, tt) for e in range(E) for fg in range(NFG) for tt in range(NTT)]
        pend = None
        wslot = [0]

        def load_w(e, fg):
            i = wslot[0] % 2
            wslot[0] += 1
            g_, rg_ = wgb[i]; u_, ru_ = wub[i]; d_, rd_ = wdb[i]
            P.dma("pool", g_[:], wg[e, :, fg * GW:(fg + 1) * GW].rearrange("(k p) f -> p k f", p=128),
                  writes=[rg_])
            P.dma("pool", u_[:], wu[e, :, fg * GW:(fg + 1) * GW].rearrange("(k p) f -> p k f", p=128),
                  writes=[ru_])
            P.dma("pool", d_[:], wd[e, fg * GW:(fg + 1) * GW, :].rearrange("(c p) d -> p c d", p=128),
                  writes=[rd_])
            return i

        def down(e, fg, tt, wi, ai):
            toks = slice(tt * TT, (tt + 1) * TT)
            d_, rd_ = wdb[wi]
            a_, ra_ = actb[ai]
            for d in range(8):
                ps, rps = pp.get()
                for fc in range(NFC):
                    P.mm(ps[:], d_[:, fc, d * 128:(d + 1) * 128], a_[:, fc, :], start=(fc == 0),
                         stop=(fc == NFC - 1), reads=[rd_, ra_[fc]], writes=[rps])
                P.op("dve", "scalar_tensor_tensor", out=hbuf[:, d, toks], in0=ps[:], scalar=col(4, d),
                     in1=hbuf[:, d, toks], op0=ALU.mult, op1=ALU.add,
                     reads=[rps, rvec, rh[tt][d]], writes=[rh[tt][d]])

        cur_w = None
        cur_key = None
        astep = 0
        cur_gb = None
        for (e, fg, tt) in steps:
            toks = slice(tt * TT, (tt + 1) * TT)
            if moe and fg == 0 and tt == 0:
                gbt, rgb = gb[e % 2]
                for t2 in range(NTT):
                    tk2 = slice(t2 * TT, (t2 + 1) * TT)
                    ps, rps = pp.get()
                    P.mm(ps[:], sel_sb[:, e * 128:(e + 1) * 128], gT[:, tk2], start=True, stop=True,
                         reads=[rsel, rgT], writes=[rps])
                    P.op("act", "activation", out=gbt[:, tk2], in_=ps[:], func=AF.Identity,
                         reads=[rps], writes=[rgb])
                cur_gb = (gbt, rgb)
            if cur_key != (e, fg):
                cur_w = load_w(e, fg)
                cur_key = (e, fg)
            g_, rg_ = wgb[cur_w]; u_, ru_ = wub[cur_w]
            ai = astep % 2
            astep += 1
            a_, ra_ = actb[ai]
            for fc in range(NFC):
                psg, rpsg = pp.get()
                for k in range(8):
                    P.mm(psg[:], g_[:, k, fc * 128:(fc + 1) * 128], mbuf[:, k, toks], start=(k == 0),
                         stop=(k == 7), reads=[rg_, rm[tt][k]], writes=[rpsg])
                psu, rpsu = pp.get()
                for k in range(8):
                    P.mm(psu[:], u_[:, k, fc * 128:(fc + 1) * 128], mbuf[:, k, toks], start=(k == 0),
                         stop=(k == 7), reads=[ru_, rm[tt][k]], writes=[rpsu])
                s_, rs_ = sil[nsil[0] % 3]
                nsil[0] += 1
                P.op("act", "activation", out=s_[:], in_=psg[:], func=AF.Silu, reads=[rpsg], writes=[rs_])
                if moe:
                    t_, rt_ = tmpb[fc % 2]
                    P.op("dve", "tensor_tensor", out=t_[:], in0=psu[:], in1=s_[:], op=ALU.mult,
                         reads=[rpsu, rs_], writes=[rt_])
                    P.op("dve", "tensor_tensor", out=a_[:, fc, :], in0=t_[:], in1=cur_gb[0][:, toks],
                         op=ALU.mult, reads=[rt_, cur_gb[1]], writes=[ra_[fc]])
                else:
                    P.op("dve", "tensor_tensor", out=a_[:, fc, :], in0=psu[:], in1=s_[:], op=ALU.mult,
                         reads=[rpsu, rs_], writes=[ra_[fc]])
            if pend is not None:
                down(*pend)
            pend = (e, fg, tt, cur_w, ai)
        down(*pend)
        for tt in range(NTT):
            toks = slice(tt * TT, (tt + 1) * TT)
            g0 = t0 + tt * TT
            if not final:
                P.dma("sp", fm(houtT)[:, :, g0:g0 + TT], hbuf[:, :, toks], reads=rh[tt], is_output=True)
            o_, ro_ = outb[nob[0] % 2]; nob[0] += 1
            emit_norm(P, c, pp, hbuf, rh[tt], toks, gsN, rgsN, lambda k: col(7, k), rvec, o_, ro_, scr,
                      otoks=slice(0, TT))
            P.dma("sp", fm(outT if final else anT)[:, :, g0:g0 + TT], o_[:], reads=ro_, is_output=True)
            if extra_kv:
                o_, ro_ = outb[nob[0] % 2]; nob[0] += 1
                emit_norm(P, c, pp, hbuf, rh[tt], toks, gsK, rgsK, lambda k: col(10, k), rvec, o_, ro_, scr,
                          otoks=slice(0, TT))
                P.dma("sp", fm(kvT)[:, :, g0:g0 + TT], o_[:], reads=ro_, is_output=True)
    P.emit()
    return nc


S_LEN = 16384


def build_ret(NTILES=S_LEN // TT):
    nc = bass.Bass("TRN2", target_bir_lowering=False)
    S = NTILES * TT
    aT = nc.dram_tensor("aT", [D, S], BF16, kind="ExternalInput").ap()
    win = nc.dram_tensor("win", [D, 1536], F32, kind="ExternalInput").ap()
    cosd = nc.dram_tensor("cosT", [128, S], F32, kind="ExternalInput").ap()
    sind = nc.dram_tensor("sinT", [128, S], F32, kind="ExternalInput").ap()
    maskd = nc.dram_tensor("maskbd", [128, 128], F32, kind="ExternalInput").ap()
    cvd = nc.dram_tensor("cvec", [128, 8], F32, kind="ExternalInput").ap()
    identd = nc.dram_tensor("identb", [128, 128], BF16, kind="ExternalInput").ap()
    ygT = nc.dram_tensor("ygT", [512, S], BF16, kind="ExternalOutput").ap()
    aTv = aT.rearrange("(k p) t -> p k t", p=128)
    ygTv = ygT.rearrange("(j p) t -> p j t", p=128)

    P = Prog(nc)
    pp = PsumPool(P, 2)
    w_sb = P.sbuf([128, 8, 1536], BF16); rw = Res()
    P.dma("pool", w_sb[:], win.rearrange("(k p) f -> p k f", p=128), writes=[rw])
    mask = P.sbuf([128, 128], F32); rmask = Res()
    P.dma("sp", mask[:], maskd[:, :], writes=[rmask])
    cv = P.sbuf([128, 8], F32); rcv = Res()
    P.dma("sp", cv[:], cvd[:, :], writes=[rcv])
    ident = P.sbuf([128, 128], BF16); rid = Res()
    P.dma("sp", ident[:], identd[:, :], writes=[rid])
    KD, QD, QD2, CDEC, NH = [cv[:, i:i + 1] for i in range(5)]

    ab = [(P.sbuf([128, 8, TT], BF16), Res()) for _ in range(2)]
    cosb = [(P.sbuf([128, TT], F32), Res()) for _ in range(2)]
    sinb = [(P.sbuf([128, TT], F32), Res()) for _ in range(2)]
    qkf = [(P.sbuf([128, TT], F32), Res()) for _ in range(4)]
    rt = [(P.sbuf([128, TT], F32), Res()) for _ in range(4)]
    qTb = [(P.sbuf([128, 2, TT], BF16), [Res(), Res()]) for _ in range(2)]
    kTb = [(P.sbuf([128, 2, TT], BF16), [Res(), Res()]) for _ in range(2)]
    vb = [(P.sbuf([128, 4, 512], BF16), [Res() for _ in range(4)]) for _ in range(2)]
    gb = [(P.sbuf([128, 4, 512], BF16), [Res() for _ in range(4)]) for _ in range(2)]
    ktm = [(P.sbuf([128, 4, 256], BF16), [Res() for _ in range(4)]) for _ in range(2)]
    pTb = [(P.sbuf([128, 128], BF16), Res()) for _ in range(2)]
    st = P.sbuf([128, 2, 512], F32); rst = [Res(), Res()]
    stb = [(P.sbuf([128, 2, 512], BF16), Res()) for _ in range(2)]
    yb = [(P.sbuf([128, 512], BF16), Res()) for _ in range(2)]
    ygb = [(P.sbuf([128, 512], BF16), Res()) for _ in range(2)]
    ygTb = [(P.sbuf([128, 4, TT], BF16), [Res() for _ in range(4)]) for _ in range(2)]
    stats = P.sbuf([128, 6], F32); rstats = Res()
    mv = P.sbuf([128, 2], F32); rmv = Res()
    vv = P.sbuf([128, 1], F32); rvv = Res()
    rs = P.sbuf([128, 1], F32); rrs = Res()
    scl = P.sbuf([128, 1], F32); rscl = Res()
    ps_kv = [(P.psum([128, 512], F32), Res()) for _ in range(2)]
    ps_s = P.psum([128, 512], F32); rps_s = Res()
    ps_t = P.psum([128, 1024], BF16); _r = Res(); rps_t = [_r, _r]

    P.op("dve", "memset", st[:], 0.0, writes=rst)
    P.op("dve", "memset", stb[1][0][:], 0.0, writes=[stb[1][1]])
    nstb = [1]

    ppo = PsumPool(P, 2)

    def proj(t):
        a, ra = ab[t % 2]
        co, rco = cosb[t % 2]
        si, rsi = sinb[t % 2]
        P.dma("sp", a[:], aTv[:, :, t * TT:(t + 1) * TT], writes=[ra])
        P.dma("sp", co[:], cosd[:, t * TT:(t + 1) * TT], writes=[rco])
        P.dma("sp", si[:], sind[:, t * TT:(t + 1) * TT], writes=[rsi])
        for j in range(4):
            ps, rps = pp.get()
            for k in range(8):
                P.mm(ps[:], w_sb[:, k, j * 128:(j + 1) * 128], a[:, k, :], start=(k == 0), stop=(k == 7),
                     reads=[rw, ra], writes=[rps])
            f, rf = qkf[j]
            P.op("act", "activation", out=f[:], in_=ps[:], func=AF.Identity, reads=[rps], writes=[rf])
            yield
        qT, rqT = qTb[t % 2]
        kT, rkT = kTb[t % 2]
        for (eng, A, B, dst, rdst, tmp) in (("dve", qkf[0], qkf[1], qT, rqT, rt[0:2]),
                                            ("pool", qkf[2], qkf[3], kT, rkT, rt[2:4])):
            (fa, rfa), (fb, rfb) = A, B
            (t1, rt1), (t2, rt2) = tmp
            P.op(eng, "tensor_tensor", out=t1[:], in0=fa[:], in1=co[:], op=ALU.mult, reads=[rfa, rco], writes=[rt1])
            P.op(eng, "tensor_tensor", out=t2[:], in0=fb[:], in1=si[:], op=ALU.mult, reads=[rfb, rsi], writes=[rt2])
            P.op(eng, "tensor_tensor", out=dst[:, 0, :], in0=t1[:], in1=t2[:], op=ALU.subtract,
                 reads=[rt1, rt2], writes=[rdst[0]])
            P.op(eng, "tensor_tensor", out=t1[:], in0=fb[:], in1=co[:], op=ALU.mult, reads=[rfb, rco], writes=[rt1])
            P.op(eng, "tensor_tensor", out=t2[:], in0=fa[:], in1=si[:], op=ALU.mult, reads=[rfa, rsi], writes=[rt2])
            P.op(eng, "tensor_tensor", out=dst[:, 1, :], in0=t1[:], in1=t2[:], op=ALU.add,
                 reads=[rt1, rt2], writes=[rdst[1]])
        v, rv = vb[t % 2]
        g, rg = gb[t % 2]
        for blk in range(4):
            bs = slice(blk * 128, (blk + 1) * 128)
            ps, rps = pp.get()
            for k in range(8):
                P.mm(ps[:], a[:, k, bs], w_sb[:, k, 512:1024], start=(k == 0), stop=(k == 7),
                     reads=[rw, ra], writes=[rps])
            P.op("act", "activation", out=v[:, blk, :], in_=ps[:], func=AF.Identity, reads=[rps], writes=[rv[blk]])
            yield
            ps, rps = pp.get()
            for k in range(8):
                P.mm(ps[:], a[:, k, bs], w_sb[:, k, 1024:1536], start=(k == 0), stop=(k == 7),
                     reads=[rw, ra], writes=[rps])
            P.op("act", "activation", out=g[:, blk, :], in_=ps[:], func=AF.Silu, reads=[rps], writes=[rg[blk]])
            yield
        km, rkm = ktm[t % 2]
        for blk in range(4):
            bs = slice(blk * 128, (blk + 1) * 128)
            for kc in range(2):
                P.op("pe", "transpose", ps_t[:, kc * 128:(kc + 1) * 128], kT[:, kc, bs], ident[:],
                     reads=[rkT[kc], rid], writes=[rps_t[0]])
            P.op("act", "activation", out=km[:, blk, :], in_=ps_t[:, 0:256], func=AF.Identity, scale=KD,
                 reads=[rps_t[0], rcv], writes=[rkm[blk]])
            yield

    def pump(gen, n):
        if gen is None:
            return
        for _ in range(n):
            try:
                next(gen)
            except StopIteration:
                return

    def recur(t, nxt):
        qT, rqT = qTb[t % 2]
        kT, rkT = kTb[t % 2]
        v, rv = vb[t % 2]
        g, rg = gb[t % 2]
        km, rkm = ktm[t % 2]
        ygT_sb, rygT = ygTb[t % 2]
        for blk in range(4):
            bs = slice(blk * 128, (blk + 1) * 128)
            for kc in range(2):
                P.mm(ps_s[:, 0:128], kT[:, kc, bs], qT[:, kc, bs], start=(kc == 0), stop=(kc == 1),
                     reads=[rkT[kc], rqT[kc]], writes=[rps_s])
            pT, rpT = pTb[blk % 2]
            P.op("dve", "tensor_tensor", out=pT[:], in0=ps_s[:, 0:128], in1=mask[:], op=ALU.mult,
                 reads=[rps_s, rmask], writes=[rpT])
            pso, rpso = ppo.get()
            P.mm(pso[:], pT[:], v[:, blk, :], start=True, stop=False, reads=[rpT, rv[blk]], writes=[rpso])
            for half in range(2):
                o = half * 64
                cs = slice(blk * 128 + o, blk * 128 + o + 64)
                sb_, rsb_ = stb[nstb[0]]
                for kc in range(2):
                    P.mm(pso[o:o + 64, :], qT[:, kc, cs], sb_[:, kc, :], start=False,
                         stop=(kc == 1), reads=[rqT[kc], rsb_], writes=[rpso])
                for kc in range(2):
                    pk, rpk = ps_kv[kc]
                    P.mm(pk[:], km[o:o + 64, blk, kc * 128:(kc + 1) * 128], v[o:o + 64, blk, :], start=True,
                         stop=True, reads=[rkm[blk], rv[blk]], writes=[rpk])
                    P.op("dve", "scalar_tensor_tensor", out=st[:, kc, :], in0=st[:, kc, :], scalar=CDEC,
                         in1=pk[:], op0=ALU.mult, op1=ALU.add, reads=[rst[kc], rcv, rpk], writes=[rst[kc]])
                nstb[0] ^= 1
                sb2, rsb2 = stb[nstb[0]]
                P.op("act", "activation", out=sb2[:], in_=st[:], func=AF.Identity, reads=rst, writes=[rsb2])
                pump(nxt, 2)
            P.op("dve", "bn_stats", out=stats[:], in_=pso[:], reads=[rpso], writes=[rstats])
            P.op("dve", "bn_aggr", out=mv[:], in_=stats[:], reads=[rstats], writes=[rmv])
            P.op("dve", "tensor_scalar", out=vv[:], in0=mv[:, 1:2], scalar1=QD2, scalar2=EPS, op0=ALU.mult,
                 op1=ALU.add, reads=[rmv, rcv], writes=[rvv])
            P.op("pool", "tensor_tensor", out=rs[:], in0=vv[:], in1=NH, op=ALU.pow, reads=[rvv, rcv],
                 writes=[rrs])
            P.op("pool", "tensor_tensor", out=scl[:], in0=rs[:], in1=QD, op=ALU.mult, reads=[rrs, rcv],
                 writes=[rscl])
            y, ry = yb[blk % 2]
            P.op("dve", "tensor_scalar", out=y[:], in0=pso[:], scalar1=mv[:, 0:1], scalar2=scl[:, 0:1],
                 op0=ALU.subtract, op1=ALU.mult, reads=[rpso, rmv, rscl], writes=[ry])
            yg, ryg = ygb[blk % 2]
            P.op("pool", "tensor_tensor", out=yg[:], in0=y[:], in1=g[:, blk, :], op=ALU.mult,
                 reads=[ry, rg[blk]], writes=[ryg])
            for j in range(4):
                P.op("pe", "transpose", ps_t[:, 512 + j * 128:512 + (j + 1) * 128], yg[:, j * 128:(j + 1) * 128],
                     ident[:], reads=[ryg, rid], writes=[rps_t[1]])
            P.op("act", "activation", out=ygT_sb[:, :, bs],
                 in_=ps_t[:, 512:1024].rearrange("p (j t) -> p j t", j=4), func=AF.Identity,
                 reads=[rps_t[1]], writes=[rygT[blk]])
        P.dma("sp", ygTv[:, :, t * TT:(t + 1) * TT], ygT_sb[:], reads=rygT, is_output=True)

    pump(proj(0), 1000)
    for t in range(NTILES):
        nxt = proj(t + 1) if t + 1 < NTILES else None
        recur(t, nxt)
        pump(nxt, 1000)
    P.emit()
    return nc


def build_attn(NQT=S_LEN // TT, stage=9, NH=4):
    nc = bass.Bass("TRN2", target_bir_lowering=False)
    S = NQT * TT
    NB = S // 128
    aT = nc.dram_tensor("aT", [D, S], BF16, kind="ExternalInput").ap()
    kvT = nc.dram_tensor("kvT", [D, S], BF16, kind="ExternalInput").ap()
    wqd = nc.dram_tensor("wq", [D, 268], F32, kind="ExternalInput").ap()
    wgd = nc.dram_tensor("wgt", [D, 256], F32, kind="ExternalInput").ap()
    wkd = nc.dram_tensor("wk", [D, 256], F32, kind="ExternalInput").ap()
    wvfd = nc.dram_tensor("wvf", [D, 260], F32, kind="ExternalInput").ap()
    bfd = nc.dram_tensor("bf", [128, 4], F32, kind="ExternalInput").ap()
    trid = nc.dram_tensor("tri", [128, 128], F32, kind="ExternalInput").ap()
    tmd = nc.dram_tensor("trimask", [128, 128], BF16, kind="ExternalInput").ap()
    idbd = nc.dram_tensor("identb", [128, 128], BF16, kind="ExternalInput").ap()
    shd = nc.dram_tensor("shiftsel", [128, 64], F32, kind="ExternalInput").ap()
    ogT = nc.dram_tensor("ogT", [256, S], BF16, kind="ExternalOutput").ap()
    fm = lambda ap: ap.rearrange("(k p) t -> p k t", p=128)
    aTv, kvTv = fm(aT), fm(kvT)

    P = Prog(nc)
    ppS = PsumPool(P, 3)
    ppB = PsumPool(P, 3)
    ps_o = [(P.psum([128, 512], F32), Res()) for _ in range(2)]

    def wload(dram, n):
        t_ = P.sbuf([128, 8, n], BF16); r_ = Res()
        P.dma("pool", t_[:], dram.rearrange("(k p) f -> p k f", p=128), writes=[r_])
        return t_, r_
    wq, rwq = wload(wqd, 268)
    wg, rwg = wload(wgd, 256)
    wk, rwk = wload(wkd, 256)
    wvf, rwvf = wload(wvfd, 260)
    bft = P.sbuf([128, 4], F32); rbf = Res()
    P.dma("sp", bft[:], bfd[:, :], writes=[rbf])
    negb = P.sbuf([128, 4], F32); rnegb = Res()
    P.op("dve", "tensor_scalar", out=negb[:], in0=bft[:], scalar1=-1.0, scalar2=None, op0=ALU.mult,
         reads=[rbf], writes=[rnegb])
    tri = P.sbuf([128, 128], F32); rtri = Res()
    P.dma("sp", tri[:], trid[:, :], writes=[rtri])
    trimask = P.sbuf([128, 128], BF16); rtm = Res()
    P.dma("sp", trimask[:], tmd[:, :], writes=[rtm])
    identb = P.sbuf([128, 128], BF16); ridb = Res()
    P.dma("sp", identb[:], idbd[:, :], writes=[ridb])
    shiftsel = P.sbuf([128, 64], F32); rshs = Res()
    P.dma("sp", shiftsel[:], shd[:, :], writes=[rshs])
    onesf = P.sbuf([128, 128], F32); ronesf = Res()
    P.op("dve", "memset", onesf[:], 1.0, writes=[ronesf])

    K_aug = P.sbuf([67, S], BF16); rK = [Res() for _ in range(NQT)]
    rKones = Res()
    P.op("dve", "memset", K_aug[:], 8.0, writes=[rKones])
    V_aug = P.sbuf([128, NB, 128], BF16); rV = [Res() for _ in range(NB)]
    rVones = Res()
    P.op("dve", "memset", V_aug[:], 1.0, writes=[rVones])
    e_all = P.sbuf([128, NB], F32); re_all = Res()
    sp_all = P.sbuf([128, NB], F32); rsp = Res()
    tot = P.sbuf([128, NB], F32); rtot = Res()
    cs = P.sbuf([128, NB], F32); rcs = Res()
    tmpF = P.sbuf([128, NB], F32); rtmpF = Res()
    negF = P.sbuf([128, NB], F32); rnegF = Res()
    Pc = P.sbuf([128, NB, 67], BF16); rPc = Res()
    P.op("dve", "memset", Pc[:], 0.0, writes=[rPc])
    r1 = P.sbuf([128, NB], F32); rr1 = Res()
    r2 = P.sbuf([128, NB], F32); rr2 = Res()
    onesNB = P.sbuf([128, NB], F32); ronesNB = Res()
    P.op("dve", "memset", onesNB[:], 1.0, writes=[ronesNB])

    kvb = [(P.sbuf([128, 8, TT], BF16), Res()) for _ in range(2)]
    ab = [(P.sbuf([128, 8, TT], BF16), Res()) for _ in range(2)]
    Qb = [(P.sbuf([67, TT], BF16), Res()) for _ in range(2)]
    pTb = [(P.sbuf([128, TT], BF16), Res()) for _ in range(3)]
    eg = P.sbuf([64, TT], F32); reg = Res()
    sgb = [(P.sbuf([64, TT], F32), Res()) for _ in range(2)]
    lr = P.sbuf([128, TT], F32); rlr = Res()
    rl = P.sbuf([64, TT], F32); rrl = Res()
    t64 = P.sbuf([64, TT], F32); rt64 = Res()
    ogb = [(P.sbuf([64, TT], BF16), Res()) for _ in range(2)]
    nkv = [0]
    na = [0]
    npt = [0]

    for h in range(NH):
        hs = slice(h * 64, (h + 1) * 64)
        for tt in range(NQT):
            kv, rkv = kvb[nkv[0] % 2]; nkv[0] += 1
            P.dma("sp", kv[:], kvTv[:, :, tt * TT:(tt + 1) * TT], writes=[rkv])
            ps, rps = ppB.get()
            for k in range(8):
                P.mm(ps[0:64, :], wk[:, k, hs], kv[:, k, :], start=(k == 0), stop=(k == 7),
                     reads=[rwk, rkv], writes=[rps])
            P.op("act", "activation", out=K_aug[0:64, tt * TT:(tt + 1) * TT], in_=ps[0:64, :], func=AF.Identity,
                 reads=[rps, rKones], writes=[rK[tt]])
            for blk in range(4):
                gb_ = tt * 4 + blk
                bs = slice(blk * 128, (blk + 1) * 128)
                ps, rps = ppB.get()
                for k in range(8):
                    P.mm(ps[:, 0:65], kv[:, k, bs], wvf[:, k, h * 65:(h + 1) * 65], start=(k == 0), stop=(k == 7),
                         reads=[rwvf, rkv], writes=[rps])
                P.op("act", "activation", out=V_aug[:, gb_, 0:64], in_=ps[:, 0:64], func=AF.Identity,
                     reads=[rps, rVones], writes=[rV[gb_]])
                P.op("act", "activation", out=e_all[:, gb_:gb_ + 1], in_=ps[:, 64:65], func=AF.Exp, scale=-1.0,
                     bias=negb[:, h:h + 1], reads=[rps, rnegb], writes=[re_all])
        P.op("act", "activation", out=sp_all[:], in_=e_all[:], func=AF.Ln, bias=onesf[:, 0:1],
             reads=[re_all, ronesf], writes=[rsp])
        psF, rpsF = ppB.get()
        P.mm(psF[:, 0:NB], tri[:], sp_all[:], start=True, stop=True, reads=[rtri, rsp], writes=[rpsF])
        psT, rpsT = ppB.get()
        P.mm(psT[:, 0:NB], onesf[:], sp_all[:], start=True, stop=True, reads=[ronesf, rsp], writes=[rpsT])
        P.op("dve", "tensor_copy", out=tot[:], in_=psT[:, 0:NB], reads=[rpsT], writes=[rtot])
        P.op("dve", "tensor_tensor_scan", out=cs[:], data0=onesNB[:], data1=tot[:], initial=0.0,
             op0=ALU.mult, op1=ALU.add, reads=[ronesNB, rtot], writes=[rcs])
        P.op("dve", "tensor_tensor", out=tmpF[:], in0=cs[:], in1=tot[:], op=ALU.subtract,
             reads=[rcs, rtot], writes=[rtmpF])
        P.op("dve", "tensor_tensor", out=negF[:], in0=psF[:, 0:NB], in1=tmpF[:], op=ALU.add,
             reads=[rpsF, rtmpF], writes=[rnegF])
        P.op("dve", "tensor_scalar", out=Pc[:, :, 64], in0=negF[:], scalar1=-1.0, scalar2=None, op0=ALU.mult,
             reads=[rnegF], writes=[rPc])
        P.op("dve", "scalar_tensor_tensor", out=r1[:], in0=negF[:], scalar=-1.0, in1=Pc[:, :, 64], op0=ALU.mult,
             op1=ALU.subtract, reads=[rnegF, rPc], writes=[rr1])
        P.op("dve", "tensor_copy", out=Pc[:, :, 65], in_=r1[:], reads=[rr1], writes=[rPc])
        P.op("dve", "tensor_tensor", out=r2[:], in0=r1[:], in1=Pc[:, :, 65], op=ALU.subtract,
             reads=[rr1, rPc], writes=[rr2])
        P.op("dve", "tensor_copy", out=Pc[:, :, 66], in_=r2[:], reads=[rr2], writes=[rPc])

        if stage == 1:
            og, rog = ogb[0]
            P.op("dve", "tensor_copy", out=og[:, 0:NB], in_=negF[0:64, :], reads=[rnegF, rPc], writes=[rog])
            P.dma("sp", ogT[h * 64:(h + 1) * 64, 0:TT], og[:], reads=[rog], is_output=True)
            continue
        steps = [(t, j) for t in range(NQT) for j in range(4 * t + 4)]
        tstate = {}
        sstate = {}
        LOOK = 2

        def qprep(t):
            a, ra = ab[na[0] % 2]
            Q, rQ = Qb[na[0] % 2]
            sg, rsg = sgb[na[0] % 2]
            og, rog = ogb[na[0] % 2]
            pso, rpso = ps_o[na[0] % 2]
            na[0] += 1
            tstate[t] = (Q, rQ, sg, rsg, og, rog, pso, rpso)
            P.dma("sp", a[:], aTv[:, :, t * TT:(t + 1) * TT], writes=[ra])
            ps, rps = ppB.get()
            for k in range(8):
                P.mm(ps[0:67, :], wq[:, k, h * 67:(h + 1) * 67], a[:, k, :], start=(k == 0), stop=False,
                     reads=[rwq, ra], writes=[rps])
            for blk in range(4):
                P.mm(ps[0:67, blk * 128:(blk + 1) * 128], Pc[:, t * 4 + blk, :], identb[:], start=False,
                     stop=(blk == 3), reads=[rPc, ridb], writes=[rps])
            P.op("act", "activation", out=Q[:], in_=ps[0:67, :], func=AF.Identity, reads=[rps], writes=[rQ])
            ps2, rps2 = ppB.get()
            for k in range(8):
                P.mm(ps2[0:64, :], wg[:, k, hs], a[:, k, :], start=(k == 0), stop=(k == 7),
                     reads=[rwg, ra], writes=[rps2])
            P.op("act", "activation", out=eg[:], in_=ps2[0:64, :], func=AF.Exp, scale=-1.0, reads=[rps2],
                 writes=[reg])
            P.op("dve", "tensor_scalar", out=eg[:], in0=eg[:], scalar1=1.0, scalar2=None, op0=ALU.add,
                 reads=[reg], writes=[reg])
            P.op("dve", "reciprocal", out=sg[:], in_=eg[:], reads=[reg], writes=[rsg])

        def emit_qk(idx):
            t, j = steps[idx]
            Q, rQ = tstate[t][0:2]
            o = j - 4 * t
            c0 = 128 * o if o > 0 else 0
            N = TT - c0
            pss, rpss = ppS.get()
            P.mm(pss[:, 0:N], K_aug[0:67, j * 128:(j + 1) * 128], Q[0:67, c0:TT], start=True, stop=True,
                 reads=[rK[j // 4], rKones, rQ], writes=[rpss])
            pT, rpT = pTb[npt[0] % 3]; npt[0] += 1
            P.op("act", "activation", out=pT[:, 0:N], in_=pss[:, 0:N], func=AF.Exp, scale=0.125,
                 bias=negF[:, j:j + 1], reads=[rpss, rnegF], writes=[rpT])
            if o >= 0:
                P.op("pool", "tensor_tensor", out=pT[:, 0:128], in0=pT[:, 0:128], in1=trimask[:], op=ALU.mult,
                     reads=[rpT, rtm], writes=[rpT])
            sstate[idx] = (pT, rpT, c0, N)

        def emit_pv(idx):
            t, j = steps[idx]
            Q, rQ, sg, rsg, og, rog, pso, rpso = tstate[t]
            pT, rpT, c0, N = sstate.pop(idx)
            nblk = 4 * t + 4
            P.mm(pso[:, c0:TT], V_aug[:, j, :], pT[:, 0:N], start=(j == 0), stop=(j == nblk - 1),
                 reads=[rV[j], rVones, rpT], writes=[rpso])
            if j == 0 and t + 1 < NQT:
                qprep(t + 1)
            if j == nblk - 1:
                P.op("act", "activation", out=lr[:], in_=pso[:], func=AF.Identity, reads=[rpso],
                     writes=[rlr])
                psb, rpsb = ppB.get()
                P.mm(psb[0:64, :], shiftsel[:], lr[:], start=True, stop=True, reads=[rshs, rlr],
                     writes=[rpsb])
                P.op("dve", "reciprocal", out=rl[:], in_=psb[0:64, :], reads=[rpsb], writes=[rrl])
                P.op("dve", "tensor_tensor", out=t64[:], in0=pso[0:64, :], in1=rl[:], op=ALU.mult,
                     reads=[rpso, rrl], writes=[rt64])
                P.op("dve", "tensor_tensor", out=og[:], in0=t64[:], in1=sg[:], op=ALU.mult, reads=[rt64, rsg],
                     writes=[rog])
                P.dma("sp", ogT[h * 64:(h + 1) * 64, t * TT:(t + 1) * TT], og[:], reads=[rog], is_output=True)
                del tstate[t]

        qprep(0)
        for idx in range(len(steps) + LOOK):
            if idx < len(steps):
                emit_qk(idx)
            if idx - LOOK >= 0:
                emit_pv(idx - LOOK)
    P.emit()
    return nc


_CACHE = {}


def _prog(key, fn):
    if key not in _CACHE:
        _CACHE[key] = fn()
    return _CACHE[key]


def _run(nc, ins):
    res = run_bass_kernel_spmd(nc, ins, core_ids=list(range(8)))
    return res.results


def _colvec(v):
    return np.ascontiguousarray(np.asarray(v, np.float32).reshape(8, 128).T)


def _ret_consts(hd, S):
    gamma = 1.0 - 2.0 ** (-5.0 - hd)
    lg = np.log(gamma)
    idx = np.arange(128)
    m = idx[:, None]
    n = idx[None, :]
    same = (m // 64) == (n // 64)
    mask = np.where(same, np.exp(lg * np.abs(n - m)) * np.exp(-lg * (n % 64 + 1)) / 16.0, 0.0)
    cv = np.zeros((128, 8), np.float32)
    cv[:, 0] = np.exp(lg * (63 - idx % 64)) / 16.0
    qd = np.exp(lg * (idx % 64 + 1))
    cv[:, 1] = qd
    cv[:, 2] = qd * qd
    cv[:, 3] = np.exp(lg * 64)
    cv[:, 4] = -0.5
    return mask.astype(np.float32), cv


def _rope_tables(S):
    inv = 1.0 / (10000.0 ** (np.arange(0, 256, 2) / 256.0))
    ang = np.arange(S)[None, :] * inv[:, None]
    return np.cos(ang).astype(np.float32), np.sin(ang).astype(np.float32)


def kernel(x, c, ada_w, ada_b, norm_g, ret_w_in, ret_w_o, kv_ada_w, kv_ada_b, kv_norm_g,
           fox_w_kv, fox_w_f, fox_b_f, fox_w_qg, fox_w_o, ffn_w_gate, ffn_w_up, ffn_w_down,
           router_w, router_b, moe_w_gate, moe_w_up, moe_w_down, final_ada_w, final_ada_b,
           final_norm_g):
    f32 = np.float32
    A = lambda a: np.asarray(a, f32)
    x, c = A(x), A(c)
    B, S, Dm = x.shape
    T = B * S
    ident_f = np.eye(128, dtype=f32)
    ident_b = ident_f.astype(NPBF)
    W_all = np.concatenate([A(ada_w[l]) for l in range(4)] + [A(kv_ada_w), A(final_ada_w)], axis=1)
    b_all = np.concatenate([A(ada_b[l]) for l in range(4)] + [A(kv_ada_b), A(final_ada_b)])
    cT = np.ascontiguousarray(c.T.reshape(8, 128, 2).transpose(1, 0, 2).reshape(128, 16))
    ins = []
    for i in range(8):
        ins.append({"cT": cT, "W": np.ascontiguousarray(W_all[:, i * 3584:(i + 1) * 3584]),
                    "bT": np.ascontiguousarray(b_all[i * 3584:(i + 1) * 3584].reshape(28, 128).T)})
    r = _run(_prog("mod", build_mod), ins)
    mods = np.concatenate([q["mod"].reshape(128, 28, 2).transpose(1, 0, 2).reshape(3584, 2) for q in r], 0)
    del W_all

    def modl(l, j, b):
        return mods[l * 6144 + j * 1024:l * 6144 + (j + 1) * 1024, b]
    kv_sh = lambda b: mods[24576:25600, b]
    kv_sc = lambda b: mods[25600:26624, b]
    f_sh = lambda b: mods[26624:27648, b]
    f_sc = lambda b: mods[27648:28672, b]
    norm_g = A(norm_g)
    zeros = np.zeros(1024, f32)

    xT = np.ascontiguousarray(x.reshape(T, Dm).T)
    ins = []
    for i in range(8):
        b = i // 4
        vec = np.concatenate([_colvec(norm_g[0, 0]), _colvec(modl(0, 1, b)), _colvec(modl(0, 0, b))], 1)
        ins.append({"hT": np.ascontiguousarray(xT[:, i * TC:(i + 1) * TC]), "vec": np.ascontiguousarray(vec)})
    r = _run(_prog("norm0", build_norm0), ins)
    aT = np.concatenate([q["aT"] for q in r], 1)
    hT = xT

    cosT, sinT = _rope_tables(S)
    idx = np.arange(128)
    tri = (idx[:, None] <= idx[None, :]).astype(f32)
    trimask = (idx[None, :] >= idx[:, None]).astype(f32).astype(NPBF)
    shs = np.zeros((128, 64), f32)
    for m_ in range(64):
        shs[m_ + 64, m_] = 1
    sel = np.zeros((8, 8, 128), f32)
    for e in range(8):
        sel[e, e, :] = 1
    sel = np.ascontiguousarray(sel.reshape(8, 1024))

    def post(l, mixT, KC, moe, final, extra_kv, wo, wg, wu, wd, rw=None, rb=None):
        FF = wg.shape[-1]
        NFC = 4 if moe else 2
        nc = _prog(("post", KC, moe, final, extra_kv), lambda: build_post(KC, moe, final, extra_kv, FF, NFC))
        ins = []
        for i in range(8):
            b = i // 4
            if final:
                ngN, scN, shN = A(final_norm_g), f_sc(b), f_sh(b)
            else:
                ngN, scN, shN = norm_g[l + 1, 0], modl(l + 1, 1, b), modl(l + 1, 0, b)
            if extra_kv:
                ngK, scK, shK = A(kv_norm_g), kv_sc(b), kv_sh(b)
            else:
                ngK, scK, shK = zeros, zeros, zeros
            groups = [modl(l, 2, b), norm_g[l, 1], modl(l, 4, b), modl(l, 3, b), modl(l, 5, b),
                      ngN, scN, shN, ngK, scK, shK]
            vec = np.ascontiguousarray(np.concatenate([_colvec(g) for g in groups], 1))
            d = {"hT": np.ascontiguousarray(hT[:, i * TC:(i + 1) * TC]),
                 "mixT": np.ascontiguousarray(mixT[:, i * TC:(i + 1) * TC]),
                 "wo": wo, "wg": wg, "wu": wu, "wd": wd, "vec": vec}
            if moe:
                d.update({"rw": rw, "rb": np.ascontiguousarray(np.tile(A(rb)[None, :], (128, 1))), "sel": sel,
                          "ident": ident_f})
            ins.append(d)
        return _run(nc, ins)

    for l in range(2):
        w_in = A(ret_w_in[l])
        ins = []
        for i in range(8):
            b, hd = i // 4, i % 4
            win = np.concatenate([w_in[:, hd * 256:(hd + 1) * 256], w_in[:, 1024 + hd * 256:1024 + (hd + 1) * 256],
                                  w_in[:, 2048 + hd * 512:2048 + (hd + 1) * 512],
                                  w_in[:, 4096 + hd * 512:4096 + (hd + 1) * 512]], 1)
            mask, cv = _ret_consts(hd, S)
            ins.append({"aT": np.ascontiguousarray(aT[:, b * S:(b + 1) * S]), "win": np.ascontiguousarray(win),
                        "cosT": cosT, "sinT": sinT, "maskbd": mask, "cvec": cv, "identb": ident_b})
        r = _run(_prog("ret", build_ret), ins)
        mixT = np.empty((2048, T), NPBF)
        for i in range(8):
            b, hd = i // 4, i % 4
            mixT[hd * 512:(hd + 1) * 512, b * S:(b + 1) * S] = r[i]["ygT"]
        if l == 0:
            r = post(0, mixT, 16, False, False, False, A(ret_w_o[0]), A(ffn_w_gate[0])[None], A(ffn_w_up[0])[None],
                     A(ffn_w_down[0])[None])
        else:
            r = post(1, mixT, 16, True, False, True, A(ret_w_o[1]), A(moe_w_gate[0]), A(moe_w_up[0]),
                     A(moe_w_down[0]), A(router_w[0]), router_b[0])
            kvT = np.concatenate([q["kvT"] for q in r], 1)
        hT = np.concatenate([q["houtT"] for q in r], 1)
        aT = np.concatenate([q["anT"] for q in r], 1)

    w_kv, w_f, b_f = A(fox_w_kv), A(fox_w_f), A(fox_b_f)
    out = None
    for j in range(2):
        l = 2 + j
        w_qg = A(fox_w_qg[j])
        ins = []
        for i in range(8):
            b, hg = i // 4, i % 4
            wq = np.zeros((1024, 268), f32)
            wvf = np.zeros((1024, 260), f32)
            for h in range(4):
                gh = hg * 4 + h
                wq[:, h * 67:h * 67 + 64] = w_qg[:, gh * 64:(gh + 1) * 64]
                wvf[:, h * 65:h * 65 + 64] = w_kv[:, 1024 + gh * 64:1024 + (gh + 1) * 64]
                wvf[:, h * 65 + 64] = w_f[:, gh]
            ins.append({"aT": np.ascontiguousarray(aT[:, b * S:(b + 1) * S]),
                        "kvT": np.ascontiguousarray(kvT[:, b * S:(b + 1) * S]),
                        "wq": wq, "wgt": np.ascontiguousarray(w_qg[:, 1024 + hg * 256:1024 + (hg + 1) * 256]),
                        "wk": np.ascontiguousarray(w_kv[:, hg * 256:(hg + 1) * 256]), "wvf": wvf,
                        "bf": np.ascontiguousarray(np.tile(b_f[None, hg * 4:(hg + 1) * 4], (128, 1))),
                        "tri": tri, "trimask": trimask, "identb": ident_b, "shiftsel": shs})
        r = _run(_prog("attn", build_attn), ins)
        mixT = np.empty((1024, T), NPBF)
        for i in range(8):
            b, hg = i // 4, i % 4
            mixT[hg * 256:(hg + 1) * 256, b * S:(b + 1) * S] = r[i]["ogT"]
        if j == 0:
            r = post(2, mixT, 8, False, False, False, A(fox_w_o[0]), A(ffn_w_gate[1])[None], A(ffn_w_up[1])[None],
                     A(ffn_w_down[1])[None])
            hT = np.concatenate([q["houtT"] for q in r], 1)
            aT = np.concatenate([q["anT"] for q in r], 1)
        else:
            r = post(3, mixT, 8, True, True, False, A(fox_w_o[1]), A(moe_w_gate[1]), A(moe_w_up[1]),
                     A(moe_w_down[1]), A(router_w[1]), router_b[1])
            outT = np.concatenate([q["outT"] for q in r], 1)
            out = np.ascontiguousarray(outT.T).reshape(B, S, Dm).astype(f32)
    return out
```
